# Optimizing a Trainium2 kernel written in Bass

```python
import jax, jax.numpy as jnp
from jax import lax
import numpy as np

D_MODEL = 1024
BATCH = 8
SEQ = 4096
DEPTH = 1

GRID_W = 64
CTX_LEN = 256
N_MOD = 6
EPS = 1e-6
MLA_HEADS = 4
MLA_Q_RANK = 256
MLA_KV_RANK = 128
MLA_NOPE = 128
MLA_ROPE = 64
MLA_V = 128
MLA_QK = MLA_NOPE + MLA_ROPE
ROPE_AXIS = MLA_ROPE // 2
ROPE_BASE = 10000.0
Q_BLOCK = 128
ML_HEADS = 4
ML_QK = 64
ML_V = 128
ML_CHUNK = 64
CONV_W = 3
FGATE_BIAS_LO = 3.0
FGATE_BIAS_HI = 6.0
MLA_COLS = MLA_Q_RANK + MLA_KV_RANK + MLA_ROPE
ML_COLS = 2 * ML_HEADS * ML_QK + 2 * ML_HEADS * ML_V + 4 * ML_HEADS
IN_COLS = MLA_COLS + ML_COLS
MIX_WIDTH = MLA_HEADS * MLA_V + ML_HEADS * ML_V
PEER_HEADS = 8
N_KEYS = 128
N_EXPERTS = N_KEYS * N_KEYS
PEER_DK = 256
PEER_DKH = PEER_DK // 2
PEER_TOPK = 16
PEER_BLOCK = 128

kernel_name = 'hybrid_mla_mlstm_peer_dit_block'


def rmsnorm(x, g):
    xf = x.astype(jnp.float32)
    y = xf * lax.rsqrt(jnp.mean(xf * xf, axis=-1, keepdims=True) + EPS)
    return (y * g.astype(jnp.float32)).astype(x.dtype)


def rope_tables(rows):
    row = jnp.repeat(jnp.arange(rows, dtype=jnp.float32), GRID_W)
    col = jnp.tile(jnp.arange(GRID_W, dtype=jnp.float32), rows)
    inv = ROPE_BASE ** (-jnp.arange(ROPE_AXIS // 2, dtype=jnp.float32) * (2.0 / ROPE_AXIS))
    ang_r = row[:, None] * inv
    ang_c = col[:, None] * inv
    return (jnp.cos(ang_r), jnp.sin(ang_r), jnp.cos(ang_c), jnp.sin(ang_c))


def rotate(t, cos, sin):
    half = t.shape[-1] // 2
    t1, t2 = t[..., :half], t[..., half:]
    cos = cos.astype(t.dtype)
    sin = sin.astype(t.dtype)
    return jnp.concatenate([t1 * cos - t2 * sin, t2 * cos + t1 * sin], axis=-1)


def apply_rope2d(t, rope):
    cos_r, sin_r, cos_c, sin_c = rope
    t_row = rotate(t[..., MLA_NOPE:MLA_NOPE + ROPE_AXIS], cos_r, sin_r)
    t_col = rotate(t[..., MLA_NOPE + ROPE_AXIS:], cos_c, sin_c)
    return jnp.concatenate([t[..., :MLA_NOPE], t_row, t_col], axis=-1)


def split_heads(t, n_heads):
    b, n, _ = t.shape
    return t.reshape(b, n, n_heads, -1).transpose(0, 2, 1, 3)


def merge_heads(t):
    b, h, n, d = t.shape
    return t.transpose(0, 2, 1, 3).reshape(b, n, h * d)


def mla_prep(cols, g_cq, w_uq, g_ckv, w_ukv, g_qn, g_kn, rope):
    b, n, _ = cols.shape
    c_q = cols[..., :MLA_Q_RANK]
    c_kv = cols[..., MLA_Q_RANK:MLA_Q_RANK + MLA_KV_RANK]
    k_rope = cols[..., MLA_Q_RANK + MLA_KV_RANK:]
    q = (rmsnorm(c_q, g_cq) @ w_uq).reshape(b, n, MLA_HEADS, MLA_QK)
    kv = (rmsnorm(c_kv, g_ckv) @ w_ukv).reshape(b, n, MLA_HEADS, MLA_NOPE + MLA_V)
    k = jnp.concatenate([kv[..., :MLA_NOPE],
                         jnp.broadcast_to(k_rope[:, :, None, :], (b, n, MLA_HEADS, MLA_ROPE))], axis=-1)
    v = kv[..., MLA_NOPE:].transpose(0, 2, 1, 3)
    q = rmsnorm(q, g_qn).transpose(0, 2, 1, 3)
    k = rmsnorm(k, g_kn).transpose(0, 2, 1, 3)
    if rope is not None:
        q = apply_rope2d(q, rope)
        k = apply_rope2d(k, rope)
    return q, k, v


def attend(q, k, v):
    s = jnp.einsum('bhqd,bhkd->bhqk', q, k).astype(jnp.float32) * (MLA_QK ** -0.5)
    p = jax.nn.softmax(s, axis=-1).astype(v.dtype)
    return jnp.einsum('bhqk,bhkd->bhqd', p, v)


def attend_blocked(q, k, v):
    b, h, n, dk = q.shape
    nb = n // Q_BLOCK
    qb = jnp.moveaxis(q.reshape(b, h, nb, Q_BLOCK, dk), 2, 0)
    ob = lax.map(lambda q_blk: attend(q_blk, k, v), qb)
    return jnp.moveaxis(ob, 0, 2).reshape(b, h, n, -1)


def mla_mixer(cols_lat, cols_ctx, g_cq, w_uq, g_ckv, w_ukv, g_qn, g_kn, rope, need_ctx_out):
    q_l, k_l, v_l = mla_prep(cols_lat, g_cq, w_uq, g_ckv, w_ukv, g_qn, g_kn, rope)
    q_c, k_c, v_c = mla_prep(cols_ctx, g_cq, w_uq, g_ckv, w_ukv, g_qn, g_kn, None)
    k_all = jnp.concatenate([k_c, k_l], axis=2)
    v_all = jnp.concatenate([v_c, v_l], axis=2)
    out_lat = merge_heads(attend_blocked(q_l, k_all, v_all))
    out_ctx = merge_heads(attend(q_c, k_c, v_c)) if need_ctx_out else None
    return out_lat, out_ctx


def centred_conv(u, w):
    k = w.shape[0]
    p = k // 2
    n = u.shape[1]
    up = jnp.pad(u, ((0, 0), (p, p), (0, 0)))
    return sum(up[:, j:j + n] * w[j] for j in range(k))


def mlstm_prep(cols, conv_w, b_i, b_f):
    b, n, _ = cols.shape
    n_qk = ML_HEADS * ML_QK
    n_v = ML_HEADS * ML_V
    qk = jax.nn.silu(centred_conv(cols[..., :2 * n_qk], conv_w))
    q = split_heads(qk[..., :n_qk], ML_HEADS).astype(jnp.float32) * (ML_QK ** -0.5)
    k = split_heads(qk[..., n_qk:], ML_HEADS).astype(jnp.float32)
    v = split_heads(cols[..., 2 * n_qk:2 * n_qk + n_v], ML_HEADS).astype(jnp.float32)
    o = cols[..., 2 * n_qk + n_v:2 * n_qk + 2 * n_v]
    gates = cols[..., 2 * n_qk + 2 * n_v:].astype(jnp.float32).reshape(b, n, 2, 2, ML_HEADS)
    ig = (gates[:, :, 0] + b_i.astype(jnp.float32)).transpose(2, 0, 3, 1)
    lf = jax.nn.log_sigmoid(gates[:, :, 1] + b_f.astype(jnp.float32)).transpose(2, 0, 3, 1)
    return q, k, v, o, ig, lf


def mlstm_scan(q, k, v, ig, lf, state, need_out):
    b, h, n_tok, _ = q.shape
    nc = n_tok // ML_CHUNK

    def chunks(a):
        return jnp.moveaxis(a.reshape(a.shape[:2] + (nc, ML_CHUNK) + a.shape[3:]), 2, 0)

    tri = jnp.tril(jnp.ones((ML_CHUNK, ML_CHUNK), dtype=bool))

    def body(carry, inp):
        c_mat, n_vec, m_prev = carry
        qc, kc, vc, ic, fc = inp
        cum_f = jnp.cumsum(fc, axis=-1)
        dmat = jnp.where(tri, cum_f[..., :, None] - cum_f[..., None, :] + ic[..., None, :], -jnp.inf)
        inter = cum_f + m_prev[..., None]
        m_t = jnp.maximum(inter, jnp.max(dmat, axis=-1))
        m_new = m_t[..., -1]
        w_s = jnp.exp(dmat[..., -1, :] - m_new[..., None])
        decay = jnp.exp(inter[..., -1] - m_new)
        c_new = decay[..., None, None] * c_mat + jnp.einsum('bhs,bhsv,bhsd->bhvd', w_s, vc, kc)
        n_new = decay[..., None] * n_vec + jnp.einsum('bhs,bhsd->bhd', w_s, kc)
        if not need_out:
            return (c_new, n_new, m_new), None
        wmat = jnp.exp(dmat - m_t[..., None]) * jnp.einsum('bhtd,bhsd->bhts', qc, kc)
        scale_inter = jnp.exp(inter - m_t)
        num = jnp.einsum('bhts,bhsv->bhtv', wmat, vc) + scale_inter[..., None] * jnp.einsum('bhvd,bhtd->bhtv', c_mat, qc)
        den = jnp.sum(wmat, axis=-1) + scale_inter * jnp.einsum('bhd,bhtd->bht', n_vec, qc)
        h_out = num / jnp.maximum(jnp.abs(den), jnp.exp(-m_t))[..., None]
        return (c_new, n_new, m_new), h_out

    state, hs = lax.scan(body, state, (chunks(q), chunks(k), chunks(v), chunks(ig), chunks(lf)))
    h_seq = jnp.moveaxis(hs, 0, 2).reshape(b, h, n_tok, -1) if need_out else None
    return state, h_seq


def mlstm_output(h_sum, o, g_out):
    b, h, n, dv = h_sum.shape
    hn = rmsnorm(h_sum.transpose(0, 2, 1, 3), g_out.reshape(h, dv)).reshape(b, n, h * dv)
    return (jax.nn.sigmoid(o.astype(jnp.float32)) * hn).astype(o.dtype)


def mlstm_mixer(cols_lat, cols_ctx, conv_w, b_i, b_f, g_out, need_ctx_out):
    q_l, k_l, v_l, o_l, ig_l, lf_l = mlstm_prep(cols_lat, conv_w, b_i, b_f)
    q_c, k_c, v_c, o_c, ig_c, lf_c = mlstm_prep(cols_ctx, conv_w, b_i, b_f)
    b = q_l.shape[0]
    outs_lat, outs_ctx = [], []
    for d in range(2):
        rev = d == 1

        def fl(a):
            return jnp.flip(a, axis=2) if rev else a

        state0 = (jnp.zeros((b, ML_HEADS, ML_V, ML_QK), jnp.float32),
                  jnp.zeros((b, ML_HEADS, ML_QK), jnp.float32),
                  jnp.zeros((b, ML_HEADS), jnp.float32))
        st_ctx, h_c = mlstm_scan(fl(q_c), fl(k_c), fl(v_c), fl(ig_c[d]), fl(lf_c[d]), state0, need_ctx_out)
        _, h_l = mlstm_scan(fl(q_l), fl(k_l), fl(v_l), fl(ig_l[d]), fl(lf_l[d]), st_ctx, True)
        outs_lat.append(fl(h_l))
        if need_ctx_out:
            outs_ctx.append(fl(h_c))
    out_lat = mlstm_output(outs_lat[0] + outs_lat[1], o_l, g_out)
    out_ctx = mlstm_output(outs_ctx[0] + outs_ctx[1], o_c, g_out) if need_ctx_out else None
    return out_lat, out_ctx


def peer(h, w_pq, sub_keys, expert_u, expert_v):
    b, n, d = h.shape
    xb_all = h.reshape(-1, PEER_BLOCK, d)

    def one(xb):
        q = (xb @ w_pq).reshape(PEER_BLOCK, PEER_HEADS, 2, PEER_DKH)
        s = jnp.einsum('thpd,hpkd->thpk', q, sub_keys).astype(jnp.float32)
        sv, si = lax.top_k(s, PEER_TOPK)
        cand = (sv[:, :, 0, :, None] + sv[:, :, 1, None, :]).reshape(PEER_BLOCK, PEER_HEADS, -1)
        cidx = (si[:, :, 0, :, None] * N_KEYS + si[:, :, 1, None, :]).reshape(PEER_BLOCK, PEER_HEADS, -1)
        best, pos = lax.top_k(cand, PEER_TOPK)
        eidx = jnp.take_along_axis(cidx, pos, axis=-1)
        g = jax.nn.softmax(best, axis=-1)
        act = jax.nn.gelu(jnp.einsum('thkd,td->thk', expert_u[eidx], xb).astype(jnp.float32), approximate=False)
        return jnp.einsum('thk,thkd->td', (g * act).astype(xb.dtype), expert_v[eidx])

    return lax.map(one, xb_all).reshape(b, n, d)


def setup_inputs(seed: int = 0) -> dict:
    key = jax.random.key(seed)
    ks = jax.random.split(key, 24)

    def nrm(k, shape, s):
        return jax.random.normal(k, shape, jnp.float32) * s

    def gain(k, shape):
        return 1.0 + nrm(k, shape, 0.01)

    D, L = D_MODEL, DEPTH
    fgate = jnp.linspace(FGATE_BIAS_LO, FGATE_BIAS_HI, ML_HEADS, dtype=jnp.float32)
    return {
        'x': nrm(ks[0], (BATCH, SEQ, D), 1.0),
        'c': nrm(ks[1], (BATCH, D), 1.0),
        'ctx': nrm(ks[2], (BATCH, CTX_LEN, D), 1.0),
        'c_ctx': nrm(ks[3], (D,), 1.0),
        'w_ada': nrm(ks[4], (L, D, N_MOD * D), 0.5 * D ** -0.5),
        'b_ada': nrm(ks[5], (L, N_MOD * D), 0.01),
        'g_norm1': gain(ks[6], (L, D)),
        'w_in': nrm(ks[7], (L, D, IN_COLS), D ** -0.5),
        'g_cq': gain(ks[8], (L, MLA_Q_RANK)),
        'w_uq': nrm(ks[9], (L, MLA_Q_RANK, MLA_HEADS * MLA_QK), MLA_Q_RANK ** -0.5),
        'g_ckv': gain(ks[10], (L, MLA_KV_RANK)),
        'w_ukv': nrm(ks[11], (L, MLA_KV_RANK, MLA_HEADS * (MLA_NOPE + MLA_V)), MLA_KV_RANK ** -0.5),
        'g_qn': gain(ks[12], (L, MLA_QK)),
        'g_kn': gain(ks[13], (L, MLA_QK)),
        'conv_qk': nrm(ks[14], (L, CONV_W, 2 * ML_HEADS * ML_QK), CONV_W ** -0.5),
        'b_igate': nrm(ks[15], (L, 2, ML_HEADS), 0.01),
        'b_fgate': fgate[None, None, :] + nrm(ks[16], (L, 2, ML_HEADS), 0.01),
        'g_mlstm': gain(ks[17], (L, ML_HEADS * ML_V)),
        'w_out': nrm(ks[18], (L, MIX_WIDTH, D), MIX_WIDTH ** -0.5),
        'g_norm2': gain(ks[19], (L, D)),
        'w_pq': nrm(ks[20], (L, D, PEER_HEADS * PEER_DK), D ** -0.5),
        'sub_keys': nrm(ks[21], (L, PEER_HEADS, 2, N_KEYS, PEER_DKH), PEER_DKH ** -0.5),
        'expert_u': nrm(ks[22], (L, N_EXPERTS, D), D ** -0.5),
        'expert_v': nrm(ks[23], (L, N_EXPERTS, D), PEER_HEADS ** -0.5),
    }


def reference(x, c, ctx, c_ctx, w_ada, b_ada, g_norm1, w_in, g_cq, w_uq, g_ckv, w_ukv, g_qn, g_kn,
              conv_qk, b_igate, b_fgate, g_mlstm, w_out, g_norm2, w_pq, sub_keys, expert_u, expert_v):
    n_lat = x.shape[1]
    ROWS = n_lat // GRID_W
    rope = rope_tables(ROWS)
    s_c = jax.nn.silu(c)
    s_cc = jax.nn.silu(c_ctx)
    for l in range(DEPTH):
        last = l == DEPTH - 1
        mod_lat = (s_c @ w_ada[l] + b_ada[l])[:, None, :]
        mod_ctx = s_cc @ w_ada[l] + b_ada[l]
        sh1, sc1, gt1, sh2, sc2, gt2 = jnp.split(mod_lat, N_MOD, axis=-1)
        csh1, csc1, cgt1, csh2, csc2, cgt2 = jnp.split(mod_ctx, N_MOD, axis=-1)
        h_lat = rmsnorm(x, g_norm1[l]) * (1.0 + sc1) + sh1
        h_ctx = rmsnorm(ctx, g_norm1[l]) * (1.0 + csc1) + csh1
        cols_lat = h_lat @ w_in[l]
        cols_ctx = h_ctx @ w_in[l]
        mla_lat, mla_ctx = mla_mixer(cols_lat[..., :MLA_COLS], cols_ctx[..., :MLA_COLS],
                                     g_cq[l], w_uq[l], g_ckv[l], w_ukv[l], g_qn[l], g_kn[l], rope, not last)
        ml_lat, ml_ctx = mlstm_mixer(cols_lat[..., MLA_COLS:], cols_ctx[..., MLA_COLS:],
                                     conv_qk[l], b_igate[l], b_fgate[l], g_mlstm[l], not last)
        x = x + gt1 * (jnp.concatenate([mla_lat, ml_lat], axis=-1) @ w_out[l])
        h2 = rmsnorm(x, g_norm2[l]) * (1.0 + sc2) + sh2
        x = x + gt2 * peer(h2, w_pq[l], sub_keys[l], expert_u[l], expert_v[l])
        if not last:
            ctx = ctx + cgt1 * (jnp.concatenate([mla_ctx, ml_ctx], axis=-1) @ w_out[l])
            hc2 = rmsnorm(ctx, g_norm2[l]) * (1.0 + csc2) + csh2
            ctx = ctx + cgt2 * peer(hc2, w_pq[l], sub_keys[l], expert_u[l], expert_v[l])
    return x
```

```python
import math
from contextlib import ExitStack

import numpy as np
import ml_dtypes
import concourse.bass as bass
import concourse.mybir as mybir
from concourse.bass_utils import run_bass_kernel_spmd

F32 = mybir.dt.float32
BF16 = mybir.dt.bfloat16
U32 = mybir.dt.uint32
I32 = mybir.dt.int32
ALU = mybir.AluOpType
AF = mybir.ActivationFunctionType
AX = mybir.AxisListType

D = 1024
SEQ = 4096
CTX = 256
NT = SEQ + CTX
NTILE = NT // 128
EPS = 1e-6
IN_COLS = 2000
CTX_OFF = 1
LAT_OFF = CTX_OFF + CTX + 1
TP = LAT_OFF + SEQ + 1

SEM_WRAP = 20000
DUMP = None


class _Op:
    __slots__ = ("eng", "fn", "deps", "signal", "tok", "is_dma", "semkey", "name", "emit_tok")

    def __init__(self, eng, fn, is_dma=False, semkey=None, name=""):
        self.eng = eng
        self.fn = fn
        self.deps = []
        self.signal = False
        self.tok = None
        self.is_dma = is_dma
        self.semkey = semkey
        self.name = name


class Prog:
    ENGS = ("pe", "act", "dve", "pool", "sp")

    def __init__(self, nc, es, same_engine_sync=("act", "dve", "pool")):
        self.nc = nc
        self.es = es
        self.ops = []
        self.state = {}
        self.same = set(same_engine_sync)
        self.sems = {}
        self.eng_count = {e: 0 for e in self.ENGS}
        self.dma_count = {}
        self.known = {e: {} for e in self.ENGS}
        self.nsem = 0
        self.last_dma = {}

    def sem(self, name):
        if name not in self.sems:
            self.sems[name] = self.es.enter_context(self.nc.semaphore("s%d" % self.nsem))
            self.nsem += 1
        return self.sems[name]

    def _cls(self, op):
        return ("dma", op.semkey) if op.is_dma else op.eng

    @classmethod
    def _k(cls, k):
        if isinstance(k, (str, int)):
            return k
        if isinstance(k, tuple):
            return tuple(cls._k(x) for x in k)
        return ("T", k.name)

    def _record(self, op, reads, writes, nowaw=False):
        reads = [self._k(k) for k in reads]
        writes = [self._k(k) for k in writes]
        deps = {}

        def add(p):
            if p is None:
                return
            deps[id(p)] = p

        for k in reads:
            st = self.state.get(k)
            if st is not None:
                add(st[0])
        for k in writes:
            st = self.state.get(k)
            if st is not None:
                add(st[0])
                for r in st[1].values():
                    add(r)
        for p in list(deps.values()):
            if p is op:
                continue
            if nowaw and p.is_dma and op.is_dma and p.semkey == op.semkey:
                continue
            if p.is_dma:
                p = self.last_dma.get(p.semkey, p)
            if (not p.is_dma) and (not op.is_dma) and p.eng == op.eng and p.eng not in self.same:
                continue
            if (not p.is_dma) and op.is_dma and p.eng == op.eng and p.eng not in self.same:
                pass
            op.deps.append(p)
            p.signal = True
        for k in reads:
            st = self.state.setdefault(k, [None, {}])
            st[1][self._cls(op)] = op
        for k in writes:
            st = self.state.get(k)
            if (nowaw and st is not None and st[0] is not None and st[0].is_dma and op.is_dma
                    and st[0].semkey == op.semkey):
                self.state[k] = [op, st[1]]
            else:
                self.state[k] = [op, {}]
        if op.is_dma:
            self.last_dma[op.semkey] = op
        self.ops.append(op)
        return op

    def op(self, eng, fn, r=(), w=(), name=""):
        return self._record(_Op(eng, fn, name=name), list(r), list(w))

    def dma(self, q, out, in_, r=(), w=(), semkey=None, nowaw=False, **kw):
        w = list(w)
        r = list(r)
        if semkey is None:
            semkey = w[0] if w else r[0]
        semkey = self._k(semkey)
        op = _Op(q, lambda e: e.dma_start(out=out, in_=in_, **kw), is_dma=True, semkey=("d", semkey))
        op.signal = True
        return self._record(op, r, w, nowaw=nowaw)

    def _assign_tokens(self, ops):
        for op in ops:
            if not op.signal:
                continue
            if op.is_dma:
                c = self.dma_count.get(op.semkey, 0) + 16
                self.dma_count[op.semkey] = c
                op.tok = (op.semkey, c, 16)
            else:
                c = self.eng_count[op.eng]
                self.eng_count[op.eng] = c + 1
                op.tok = ((op.eng, c // SEM_WRAP), c % SEM_WRAP + 1, 1)

    def flush(self, final_wait_keys=()):
        ops = self.ops
        self.ops = []
        finals = []
        for k in final_wait_keys:
            st = self.state.get(self._k(k))
            if st is not None and st[0] is not None:
                st[0].signal = True
                finals.append(st[0])
        per_eng = {e: [] for e in self.ENGS}
        for op in ops:
            per_eng[op.eng].append(op)
        for e in self.ENGS:
            for op in reversed(per_eng[e]):
                if not op.is_dma:
                    op.signal = True
                    break
        self._assign_tokens(ops)
        self._back = []
        for e in self.ENGS:
            last = None
            for op in reversed(per_eng[e]):
                if op.is_dma:
                    continue
                if op.tok is not None:
                    last = op.tok
                    op.emit_tok = True
                else:
                    op.tok = last
                    op.emit_tok = False
        nc = self.nc
        for op in ops:
            if op.tok is not None:
                self.sem(op.tok[0])

        def run(engname, eng):
            known = self.known[engname]
            for op in per_eng[engname]:
                for p in op.deps:
                    s, v, _ = p.tok
                    if known.get(s, 0) >= v:
                        continue
                    eng.wait_ge(self.sems[s], v)
                    known[s] = v
                if DUMP is not None:
                    DUMP.append((engname, op.name, [p.tok for p in op.deps], op.tok, (op.is_dma or op.emit_tok)))
                ins = op.fn(eng)
                if op.is_dma or op.emit_tok:
                    ins.then_inc(self.sems[op.tok[0]], op.tok[2])
            for e2 in self.ENGS:
                if e2 == engname:
                    continue
                for op2 in reversed(per_eng[e2]):
                    if (not op2.is_dma) and op2.tok is not None:
                        s2, v2, _ = op2.tok
                        if known.get(s2, 0) < v2:
                            eng.wait_ge(self.sems[s2], v2)
                            known[s2] = v2
                        break
            for sk, cnt_ in self.dma_count.items():
                if known.get(sk, 0) < cnt_:
                    eng.wait_ge(self.sems[sk], cnt_)
                    known[sk] = cnt_
            if engname == "sp":
                for p in finals:
                    s, v, _ = p.tok
                    if known.get(s, 0) < v:
                        eng.wait_ge(self.sems[s], v)
                        known[s] = v

        with nc.Block() as block:
            @block.tensor
            def _(e):
                run("pe", e)

            @block.scalar
            def _(e):
                run("act", e)

            @block.vector
            def _(e):
                run("dve", e)

            @block.gpsimd
            def _(e):
                run("pool", e)

            @block.sync
            def _(e):
                run("sp", e)


MLA_COLS = 448
C_Q0, C_KV0, C_KR0 = 0, 256, 384
ML_Q0, ML_K0, ML_V0, ML_O0, ML_G0 = 448, 704, 960, 1472, 1984


def _swap_idx():
    idx = np.arange(64)
    half = (idx % 32) // 16
    return np.where(half == 0, idx + 16, idx - 16)


def _host_consts():
    c = {}
    c["ident"] = np.eye(128, dtype=np.float32)
    inv = (10000.0 ** (-np.arange(16, dtype=np.float32) * (2.0 / 32))).astype(np.float32)
    t = np.arange(SEQ)
    row = (t // 64).astype(np.float32)
    col = (t % 64).astype(np.float32)
    cosT = np.ones((64, NT), np.float32)
    sinT = np.zeros((64, NT), np.float32)
    for d in range(64):
        pos = row if d < 32 else col
        half = (d % 32) // 16
        ang = (pos * inv[d % 16]).astype(np.float32)
        cosT[d, CTX:] = np.cos(ang)
        sinT[d, CTX:] = np.sin(ang) * (-1.0 if half == 0 else 1.0)
    c["cosT"] = cosT
    c["sinT"] = sinT
    s_i = np.arange(128)[:, None]
    t_i = np.arange(128)[None, :]
    c["mask_f"] = (s_i <= t_i).astype(np.float32)
    c["mask_b"] = (s_i >= t_i).astype(np.float32)
    c["iota"] = np.tile(np.arange(128, dtype=np.float32)[None, :], (128, 1))
    return c


def build_program(debug=None, stop_after=None, d_steps=NTILE, e_blocks=8, elevel=9, f_groups=8, g_sgs=4, g_scs=16):
    debug = debug or set()
    nc = bass.Bass("TRN2", target_bir_lowering=False)
    es = ExitStack()
    P = Prog(nc, es)

    def din(name, shape, dt=F32):
        return nc.dram_tensor(name, list(shape), dt, kind="ExternalInput").ap()

    def dscratch(name, shape, dt):
        kind = "ExternalOutput" if name in debug else "Internal"
        return nc.dram_tensor(name, list(shape), dt, kind=kind).ap()

    x_d = din("x", [SEQ, D])
    ctx_d = din("ctx", [CTX, D])
    c_d = din("c", [D])
    cctx_d = din("c_ctx", [D])
    w_ada_d = din("w_ada", [D, 6 * D])
    b_ada_d = din("b_ada", [6 * D])
    g1_d = din("g_norm1", [D])
    w_in_d = din("w_in", [D, IN_COLS])
    g_cq_d = din("g_cq", [256])
    w_uq_d = din("w_uq", [256, 768])
    g_ckv_d = din("g_ckv", [128])
    w_ukv_d = din("w_ukv", [128, 1024])
    g_qn_d = din("g_qn", [192])
    g_kn_d = din("g_kn", [192])
    conv_d = din("conv_qk", [3, 512])
    b_ig_d = din("b_igate", [8])
    b_fg_d = din("b_fgate", [8])
    g_ml_d = din("g_mlstm", [512])
    w_out_d = din("w_out", [D, D])
    w_pq_d = din("w_pq", [D, 2048])
    subk_d = din("sub_keys", [16, 128, 128])
    eu_d = din("expert_u", [16384, D])
    ev_d = din("expert_v", [16384, D])
    iota_d = din("iota", [128, 128])
    g2_d = din("g_norm2", [D])
    maskf_d = din("mask_f", [128, 128])
    maskb_d = din("mask_b", [128, 128])
    ident_d = din("ident", [128, 128])
    cosT_d = din("cosT", [64, NT])
    sinT_d = din("sinT", [64, NT])
    out_d = nc.dram_tensor("out", [SEQ, D], F32, kind="ExternalOutput").ap()

    KnT_d = dscratch("KnT_d", [4, 128, NT], BF16)
    KrT_d = dscratch("KrT_d", [4, 64, NT], BF16)
    Vt_d = dscratch("Vt_d", [NT, 512], BF16)
    QnT_d = dscratch("QnT_d", [4, 128, SEQ], BF16)
    QrT_d = dscratch("QrT_d", [4, 64, SEQ], BF16)
    mqT_d = dscratch("mqT_d", [4, 64, NT], BF16)
    mkT_d = dscratch("mkT_d", [4, 64, NT], BF16)
    mk_d = dscratch("mk_d", [NT, 256], BF16)
    mv_d = dscratch("mv_d", [NT, 512], BF16)
    mo_d = dscratch("mo_d", [SEQ, 512], BF16)
    mg_d = dscratch("mg_d", [NT, 16], F32)
    mixT_d = dscratch("mixT_d", [8, 128, SEQ], BF16)
    x1_d = out_d
    h2T_d = dscratch("h2T_d", [8, 128, SEQ], BF16)
    W_d = dscratch("W_d", [32, 2, 128, 128, 64], BF16)
    UT_d = dscratch("UT_d", [16, 128, 8, 1024], BF16)

    hT_dbg = dscratch("hT_dbg", [128, 8, TP], BF16) if "hT_dbg" in debug else None
    mod_dbg = dscratch("mod_dbg", [128, 6 * D], F32) if "mod_dbg" in debug else None

    def sb(name, shape, dt):
        return es.enter_context(nc.sbuf_tensor(name, list(shape), dt))

    def psum(name, shape, dt=F32):
        return es.enter_context(nc.psum_tensor(name, list(shape), dt))

    ident_f = sb("ident_f", [128, 128], F32)
    ident_b = sb("ident_b", [128, 128], BF16)
    ones_f = sb("ones_f", [128, 128], F32)
    ones_b = sb("ones_b", [128, 128], BF16)
    epsc = sb("epsc", [128, 4], F32)
    modrep = sb("modrep", [128, 6 * D], F32)
    cols1 = sb("cols1", [128, 4, 8], F32)
    PS = [psum("ps%d" % i, [128, 512], F32) for i in range(8)]
    ps_rr = [0]

    def next_ps():
        ps_rr[0] = (ps_rr[0] + 1) % 3
        return PS[5 + ps_rr[0]]

    P.dma("sp", ident_f[:], ident_d, w=[ident_f])
    P.op("dve", lambda e: e.tensor_copy(out=ident_b[:], in_=ident_f[:]), r=[ident_f], w=[ident_b])
    P.op("dve", lambda e: e.memset(ones_f[:], 1.0), w=[ones_f])
    P.op("dve", lambda e: e.memset(ones_b[:], 1.0), w=[ones_b])
    for i, v in enumerate((EPS, 256 * EPS, 128 * EPS, 192 * EPS)):
        P.op("dve", (lambda e, i=i, v=v: e.memset(epsc[:, i:i + 1], v)), w=[(epsc, i)])
    epskeys = [(epsc, i) for i in range(4)]

    esA = ExitStack()

    def sbA(name, shape, dt):
        return esA.enter_context(nc.sbuf_tensor(name, list(shape), dt))

    modrep_c = sbA("modrep_c", [128, 2 * D], F32)
    cvec = sbA("cvec", [128, 2, 8], F32)
    svec = sbA("svec", [128, 2, 8], F32)
    srep = sbA("srep", [128, 2, 8, 128], F32)
    brow = sbA("brow", [1, 6 * D], F32)
    g1row = sbA("g1row", [1, D], F32)
    g1rep = sbA("g1rep", [128, D], F32)
    wch = [sbA("wch%d" % i, [128, 8, 512], F32) for i in range(2)]
    tmpA = sbA("tmpA", [128, D], F32)

    P.dma("sp", cvec[:, 0, :], c_d.rearrange("(p k) -> p k", k=8), w=[cvec], semkey="misc_ld", nowaw=True)
    P.dma("sp", cvec[:, 1, :], cctx_d.rearrange("(p k) -> p k", k=8), w=[cvec], semkey="misc_ld", nowaw=True)
    P.dma("sp", brow[:], b_ada_d.rearrange("(o n) -> o n", o=1), w=[brow], semkey="misc_ld", nowaw=True)
    P.dma("sp", g1row[:], g1_d.rearrange("(o n) -> o n", o=1), w=[g1row], semkey="misc_ld", nowaw=True)
    P.op("act", lambda e: e.activation(out=svec[:], in_=cvec[:], func=AF.Silu), r=[cvec], w=[svec])
    P.op("dve", lambda e: e.tensor_copy(out=srep[:], in_=svec[:].unsqueeze(3).to_broadcast([128, 2, 8, 128])),
         r=[svec], w=[srep])
    w_ada_v = w_ada_d.rearrange("(p k) n -> p k n", k=8)
    NCH = 12
    for j in range(NCH):
        wb = wch[j % 2]
        P.dma("sp" if j % 2 == 0 else "act", wb[:], w_ada_v[:, :, j * 512:(j + 1) * 512], w=[wb])
        nvar = 2 if j < 4 else 1
        for v in range(nvar):
            pt = PS[(2 * j + v) % 4]
            for kc in range(8):
                P.op("pe", (lambda e, pt=pt, v=v, kc=kc, wb=wb: e.matmul(pt[:], lhsT=srep[:, v, kc, :], rhs=wb[:, kc, :],
                                                                    start=(kc == 0), stop=False)),
                     r=[srep, wb], w=[pt])
            P.op("pe", (lambda e, pt=pt, j=j: e.matmul(pt[:], lhsT=ones_f[0:1, :], rhs=brow[0:1, j * 512:(j + 1) * 512],
                                                       start=False, stop=True)),
                 r=[ones_f, brow], w=[pt])
            dst = modrep if v == 0 else modrep_c
            P.op("act" if v == 0 else "dve",
                 (lambda e, pt=pt, dst=dst, j=j, v=v: (e.activation(out=dst[:, j * 512:(j + 1) * 512], in_=pt[:], func=AF.Copy)
                                                      if v == 0 else e.tensor_copy(out=dst[:, j * 512:(j + 1) * 512], in_=pt[:]))),
                 r=[pt], w=[(dst, j)])
    for h in range(2):
        pt = PS[4 + h]
        P.op("pe", (lambda e, pt=pt, h=h: e.matmul(pt[:], lhsT=ones_f[0:1, :], rhs=g1row[0:1, h * 512:(h + 1) * 512],
                                                   start=True, stop=True)), r=[ones_f, g1row], w=[pt])
        P.op("dve", (lambda e, pt=pt, h=h: e.tensor_copy(out=g1rep[:, h * 512:(h + 1) * 512], in_=pt[:])),
             r=[pt], w=[(g1rep, h)])

    def diag_extract(dst_ap, src_ap, rkeys, wkey, tmp):
        P.op("dve", lambda e: e.tensor_tensor(out=tmp[:].rearrange("p (c j) -> p c j", j=128),
                                              in0=src_ap.rearrange("p (c j) -> p c j", j=128),
                                              in1=ident_f[:].unsqueeze(1).to_broadcast([128, 8, 128]), op=ALU.mult),
             r=list(rkeys) + [ident_f], w=[tmp])
        P.op("dve", lambda e: e.tensor_reduce(out=dst_ap, in_=tmp[:].rearrange("p (c j) -> p c j", j=128), axis=AX.X, op=ALU.add),
             r=[tmp], w=[wkey])

    Grep = sbA("Grep", [128, D], F32)
    modkeys = [(modrep, j) for j in range(NCH)]
    modckeys = [(modrep_c, j) for j in range(4)]
    g1keys = [(g1rep, 0), (g1rep, 1)]
    P.op("dve", lambda e: e.scalar_tensor_tensor(out=Grep[:], in0=modrep[:, D:2 * D], scalar=1.0, in1=g1rep[:], op0=ALU.add, op1=ALU.mult),
         r=modkeys + g1keys, w=[Grep])
    diag_extract(cols1[:, 0, :], Grep[:], [Grep], (cols1, 0), tmpA)
    diag_extract(cols1[:, 1, :], modrep[:, 0:D], modkeys, (cols1, 1), tmpA)
    P.op("dve", lambda e: e.scalar_tensor_tensor(out=Grep[:], in0=modrep_c[:, D:2 * D], scalar=1.0, in1=g1rep[:], op0=ALU.add, op1=ALU.mult),
         r=modckeys + g1keys, w=[Grep])
    diag_extract(cols1[:, 2, :], Grep[:], [Grep], (cols1, 2), tmpA)
    diag_extract(cols1[:, 3, :], modrep_c[:, 0:D], modckeys, (cols1, 3), tmpA)
    if mod_dbg is not None:
        P.dma("sp", mod_dbg, modrep[:], r=modkeys, w=["mod_dbg"])
    P.flush()
    esA.close()

    esH = ExitStack()
    hT = esH.enter_context(nc.sbuf_tensor("hT", [128, 8, TP], BF16))
    esB = ExitStack()

    def sbB(name, shape, dt):
        return esB.enter_context(nc.sbuf_tensor(name, list(shape), dt))

    xt = [sbB("xt%d" % i, [128, 4, D], F32) for i in range(2)]
    xs = [sbB("xs%d" % i, [128, 4, D], BF16) for i in range(2)]
    junk = sbB("junk", [128, D], BF16)
    ss = [sbB("ss%d" % i, [128, 4], F32) for i in range(2)]
    rstd = [sbB("rstd%d" % i, [128, 4], F32) for i in range(2)]

    zcols = (0, CTX_OFF + CTX, TP - 1)
    for z in zcols:
        P.op("pool", (lambda e, z=z: e.memset(hT[:, :, z:z + 1], 0.0)), w=[(hT, "z%d" % z)])

    groups = [("ctx", 0, 2)] + [("lat", g * 4, 4) for g in range(8)]
    x_v = x_d.rearrange("(n p) d -> p n d", p=128)
    ctx_v = ctx_d.rearrange("(n p) d -> p n d", p=128)

    def load_group(gi):
        seg, t0, n = groups[gi]
        src = (ctx_v if seg == "ctx" else x_v)[:, t0:t0 + n, :]
        P.dma("sp", xt[gi % 2][:, 0:n, :], src, w=[xt[gi % 2]])

    load_group(0)
    for gi, (seg, t0, n) in enumerate(groups):
        if gi + 1 < len(groups):
            load_group(gi + 1)
        X = xt[gi % 2]
        XS = xs[gi % 2]
        SS = ss[gi % 2]
        RS = rstd[gi % 2]
        for i in range(n):
            P.op("act", (lambda e, X=X, SS=SS, i=i: e.activation(out=junk[:], in_=X[:, i, :], func=AF.Square, accum_out=SS[:, i:i + 1])),
                 r=[X], w=[junk, (SS, i)])
        P.op("act", (lambda e, SS=SS, RS=RS, n=n: e.activation(out=RS[:, 0:n], in_=SS[:, 0:n], func=AF.Sqrt, bias=epsc[:, 0:1], scale=1.0 / D)),
             r=[(SS, i) for i in range(n)] + epskeys, w=[RS])
        P.op("dve", (lambda e, RS=RS, n=n: e.reciprocal(out=RS[:, 0:n], in_=RS[:, 0:n])), r=[RS], w=[RS])
        for i in range(n):
            P.op("dve", (lambda e, X=X, XS=XS, RS=RS, i=i: e.tensor_scalar(out=XS[:, i, :], in0=X[:, i, :], scalar1=RS[:, i:i + 1], scalar2=None, op0=ALU.mult)),
                 r=[X, RS], w=[(XS, i)])
        cbase = 0 if seg == "lat" else 2
        off = (LAT_OFF if seg == "lat" else CTX_OFF) + t0 * 128
        for kc in range(8):
            pt = PS[kc % 4]
            ptb = pt[:].bitcast(BF16)
            for i in range(n):
                P.op("pe", (lambda e, ptb=ptb, XS=XS, i=i, kc=kc: e.transpose(out=ptb[:, i * 128:(i + 1) * 128], in_=XS[:, i, kc * 128:(kc + 1) * 128], identity=ident_b[:])),
                     r=[(XS, i), ident_b], w=[pt])
            if kc % 2 == 0:
                P.op("act", (lambda e, ptb=ptb, kc=kc, off=off, n=n, cbase=cbase: e.activation(
                    out=hT[:, kc, off:off + n * 128], in_=ptb[:, 0:n * 128], func=AF.Identity,
                    scale=cols1[:, cbase, kc:kc + 1], bias=cols1[:, cbase + 1, kc:kc + 1])),
                    r=[pt, (cols1, cbase), (cols1, cbase + 1)], w=[(hT, gi, kc)])
            else:
                P.op("dve", (lambda e, ptb=ptb, kc=kc, off=off, n=n, cbase=cbase: e.tensor_scalar(
                    out=hT[:, kc, off:off + n * 128], in0=ptb[:, 0:n * 128],
                    scalar1=cols1[:, cbase, kc:kc + 1], scalar2=cols1[:, cbase + 1, kc:kc + 1], op0=ALU.mult, op1=ALU.add)),
                    r=[pt, (cols1, cbase), (cols1, cbase + 1)], w=[(hT, gi, kc)])
    hkeys = [(hT, gi, kc) for gi in range(len(groups)) for kc in range(8)] + [(hT, "z%d" % z) for z in zcols]
    if hT_dbg is not None:
        P.dma("sp", hT_dbg, hT[:], r=hkeys, w=["hT_dbg"])
    P.flush()
    esB.close()
    if stop_after == "B":
        P.op("sp", lambda e: e.nop(), r=["hT_dbg", "mod_dbg"])
        P.flush(final_wait_keys=["hT_dbg", "mod_dbg"])
        esH.close()
        es.close()
        return nc

    esC = ExitStack()

    def sbC(name, shape, dt):
        return esC.enter_context(nc.sbuf_tensor(name, list(shape), dt))

    w_in_b = sbC("w_in_b", [128, 8, IN_COLS], BF16)
    wtap = sbC("wtap", [128, 3, 8, 512], BF16)
    wkr_sw = sbC("wkr_sw", [128, 8, 64], BF16)
    w_uq_b = sbC("w_uq_b", [128, 2, 768], BF16)
    w_uq_sw = sbC("w_uq_sw", [128, 2, 4, 64], BF16)
    w_ukv_b = sbC("w_ukv_b", [128, 1024], BF16)
    cosT = sbC("cosT_s", [64, NT], BF16)
    sinT = sbC("sinT_s", [64, NT], BF16)
    convrep = sbC("convrep", [128, 3, 512], F32)
    gcol = sbC("gcol", [128, 16], F32)
    brow16 = sbC("brow16", [1, 16], F32)

    w_in_v = w_in_d.rearrange("(k p) n -> p k n", p=128)
    for kc in range(8):
        P.dma("pool", w_in_b[:, kc, :], w_in_v[:, kc, :], w=[(w_in_b, kc)], semkey="w_in_ld", nowaw=True)
    winkeys = [(w_in_b, kc) for kc in range(8)]
    P.dma("pool", w_uq_b[:], w_uq_d.rearrange("(k p) n -> p k n", p=128), w=[w_uq_b], semkey="misc_ld", nowaw=True)
    P.dma("pool", w_ukv_b[:], w_ukv_d, w=[w_ukv_b], semkey="misc_ld", nowaw=True)
    P.dma("pool", cosT[:], cosT_d, w=[cosT], semkey="misc_ld", nowaw=True)
    P.dma("pool", sinT[:], sinT_d, w=[sinT], semkey="misc_ld", nowaw=True)
    def colsrc(d, a0, n_):
        return d[a0:a0 + n_].rearrange("(p o) -> p o", o=1)
    gsrc = [(0, g_cq_d, 0, 128, 0), (1, g_cq_d, 128, 128, 0), (2, g_ckv_d, 0, 128, 0),
            (3, g_qn_d, 0, 128, 0), (4, g_qn_d, 128, 64, 0), (6, g_kn_d, 0, 128, 0), (7, g_kn_d, 128, 64, 0)]
    for (d0, s0) in [(0, 16), (16, 0), (32, 48), (48, 32)]:
        gsrc.append((5, g_qn_d, 128 + s0, 16, d0))
        gsrc.append((8, g_kn_d, 128 + s0, 16, d0))
    P.op("dve", lambda e: e.memset(gcol[:], 0.0), w=[gcol])
    for gi_, (ci, d, a0, n_, p0) in enumerate(gsrc):
        P.dma("sp", gcol[p0:p0 + n_, ci:ci + 1], colsrc(d, a0, n_), r=[gcol], w=[(gcol, "ld", gi_)], semkey="gcol_ld", nowaw=True)
    gldkeys = [(gcol, "ld", gi_) for gi_ in range(len(gsrc))]
    GMUL = {0: 16.0, 1: 16.0, 2: math.sqrt(128.0), 6: math.sqrt(192.0), 7: math.sqrt(192.0), 8: math.sqrt(192.0)}
    for ci, mul in GMUL.items():
        P.op("dve", (lambda e, ci=ci, mul=mul: e.tensor_scalar(out=gcol[:, ci:ci + 1], in0=gcol[:, ci:ci + 1], scalar1=mul, scalar2=None, op0=ALU.mult)),
             r=gldkeys, w=[(gcol, ci)])
    gkeys = gldkeys + [(gcol, ci) for ci in GMUL]
    P.dma("sp", brow16[0:1, 0:8], b_ig_d.rearrange("(o n) -> o n", o=1), w=[(brow16, 0)], semkey="misc_ld", nowaw=True)
    P.dma("sp", brow16[0:1, 8:16], b_fg_d.rearrange("(o n) -> o n", o=1), w=[(brow16, 1)], semkey="misc_ld", nowaw=True)
    P.dma("sp", convrep[:], conv_d.rearrange("(o j) n -> o j n", o=1).to_broadcast([128, 3, 512]), w=[convrep], semkey="misc_ld", nowaw=True)
    for j in range(3):
        P.op("dve", (lambda e, j=j: e.tensor_tensor(out=wtap[:, j, :, :], in0=w_in_b[:, :, ML_Q0:ML_Q0 + 512],
                                                    in1=convrep[:, j, :].unsqueeze(1).to_broadcast([128, 8, 512]), op=ALU.mult)),
             r=winkeys + [convrep], w=[(wtap, j)])
    tapkeys = [(wtap, j) for j in range(3)]
    sw_blocks = [(0, 16), (16, 0), (32, 48), (48, 32)]
    for (d0, s0) in sw_blocks:
        P.op("pool", (lambda e, d0=d0, s0=s0: e.tensor_copy(out=wkr_sw[:, :, d0:d0 + 16], in_=w_in_b[:, :, C_KR0 + s0:C_KR0 + s0 + 16])),
             r=winkeys, w=[(wkr_sw, d0)])
        P.op("pool", (lambda e, d0=d0, s0=s0: e.tensor_copy(
            out=w_uq_sw[:, :, :, d0:d0 + 16],
            in_=w_uq_b[:].rearrange("p k (h c) -> p k h c", c=192)[:, :, :, 128 + s0:128 + s0 + 16])),
            r=[w_uq_b], w=[(w_uq_sw, d0)])
    krswkeys = [(wkr_sw, d0) for d0, _ in sw_blocks]
    uqswkeys = [(w_uq_sw, d0) for d0, _ in sw_blocks]

    sqA = [sbC("sqA%d" % i, [128, 512], BF16) for i in range(3)]
    rrep = [sbC("rrep%d" % i, [128, 512], F32) for i in range(2)]
    cqn = sbC("cqn", [128, 2, 512], BF16)
    ckvn = sbC("ckvn", [128, 512], BF16)
    krsq = sbC("krsq", [64, 512], BF16)
    onT = [sbC("onT%d" % i, [128, 512], BF16) for i in range(2)]
    orT = [sbC("orT%d" % i, [64, 512], BF16) for i in range(2)]
    t1 = sbC("t1", [64, 512], F32)
    t2 = sbC("t2", [64, 512], F32)
    fmT = [sbC("fmT%d" % i, [128, 512], BF16) for i in range(2)]
    tokb = [sbC("tokb%d" % i, [128, 512], BF16) for i in range(3)]
    gat = [sbC("gat%d" % i, [128, 16], F32) for i in range(2)]
    cnt = {"sq": 0, "rr": 0, "on": 0, "or": 0, "fm": 0, "tok": 0, "gat": 0}

    def rot(lst, key):
        cnt[key] += 1
        return lst[cnt[key] % len(lst)]

    blocks = [("ctx", 0, 256)] + [("lat", b * 512, 512) for b in range(8)]

    def hcols(seg, t0, n, kc, shift=0):
        off = (LAT_OFF if seg == "lat" else CTX_OFF) + t0 + shift
        return hT[:, kc, off:off + n]

    def proj8(pt_ap, wfn, seg, t0, n, rkeys, wkey, taps=False):
        k = 0
        tot = 24 if taps else 8
        for j in (range(3) if taps else [1]):
            for kc in range(8):
                P.op("pe", (lambda e, j=j, kc=kc, k=k: e.matmul(pt_ap, lhsT=wfn(j, kc), rhs=hcols(seg, t0, n, kc, j - 1),
                                                             start=(k == 0), stop=(k == tot - 1))),
                     r=list(rkeys) + hkeys, w=[wkey])
                k += 1

    def rsqrt_rep(pt, n, eps_col, dst):
        P.op("act", (lambda e: e.activation(out=dst[:, 0:n], in_=pt[:, 0:n], func=AF.Sqrt, bias=epsc[:, eps_col:eps_col + 1], scale=1.0)),
             r=[pt] + epskeys, w=[dst])
        P.op("dve", (lambda e: e.reciprocal(out=dst[:, 0:n], in_=dst[:, 0:n])), r=[dst], w=[dst])

    def head_prep(ptn, ptr, ptrs, sq_r_tile, gn, gr, grs, n, tg0, dstn_d, dstr_d):
        sqn = rot(sqA, "sq")
        P.op("act", (lambda e: e.activation(out=sqn[:, 0:n], in_=ptn[:, 0:n], func=AF.Square)), r=[ptn], w=[sqn])
        pss = next_ps()
        P.op("pe", (lambda e: e.matmul(pss[:, 0:n], lhsT=ones_b[:, :], rhs=sqn[:, 0:n], start=True, stop=False)), r=[ones_b, sqn], w=[pss])
        P.op("pe", (lambda e: e.matmul(pss[:, 0:n], lhsT=ones_b[0:64, :], rhs=sq_r_tile[0:64, 0:n], start=False, stop=True)),
             r=[ones_b, sq_r_tile], w=[pss])
        rr = rot(rrep, "rr")
        rsqrt_rep(pss, n, 3, rr)
        on = rot(onT, "on")
        P.op("dve", (lambda e: e.scalar_tensor_tensor(out=on[:, 0:n], in0=ptn[:, 0:n], scalar=gcol[:, gn:gn + 1], in1=rr[:, 0:n], op0=ALU.mult, op1=ALU.mult)),
             r=[ptn, rr] + gkeys, w=[on])
        P.dma("sp", dstn_d, on[:, 0:n], r=[on], w=["mla_scr"], nowaw=True)
        orr = rot(orT, "or")
        P.op("dve", (lambda e: e.scalar_tensor_tensor(out=t1[:, 0:n], in0=ptr[0:64, 0:n], scalar=gcol[0:64, gr:gr + 1], in1=cosT[:, tg0:tg0 + n], op0=ALU.mult, op1=ALU.mult)),
             r=[ptr, cosT] + gkeys, w=[t1])
        P.op("dve", (lambda e: e.scalar_tensor_tensor(out=t2[:, 0:n], in0=ptrs[0:64, 0:n], scalar=gcol[0:64, grs:grs + 1], in1=sinT[:, tg0:tg0 + n], op0=ALU.mult, op1=ALU.mult)),
             r=[ptrs, sinT] + gkeys, w=[t2])
        P.op("pool", (lambda e: e.tensor_tensor(out=t1[:, 0:n], in0=t1[:, 0:n], in1=t2[:, 0:n], op=ALU.add)), r=[t1, t2], w=[t1])
        P.op("dve", (lambda e: e.tensor_tensor(out=orr[:, 0:n], in0=t1[:, 0:n], in1=rr[0:64, 0:n], op=ALU.mult)), r=[t1, rr], w=[orr])
        P.dma("sp", dstr_d, orr[:, 0:n], r=[orr], w=["mla_scr"], nowaw=True)

    def block_body(bi, seg, t0, n):
        tg0 = t0 if seg == "ctx" else CTX + t0
        ntile = n // 128
        pq = [PS[0], PS[1]]
        if seg == "lat":
            for cc in range(2):
                proj8(pq[cc][:, 0:n], (lambda j, kc, cc=cc: w_in_b[:, kc, C_Q0 + cc * 128:C_Q0 + (cc + 1) * 128]), seg, t0, n, winkeys, pq[cc])
        pkv = PS[2]
        proj8(pkv[:, 0:n], (lambda j, kc: w_in_b[:, kc, C_KV0:C_KV0 + 128]), seg, t0, n, winkeys, pkv)
        pkr = PS[3]
        proj8(pkr[0:64, 0:n], (lambda j, kc: w_in_b[:, kc, C_KR0:C_KR0 + 64]), seg, t0, n, winkeys, pkr)
        pkrs = PS[4]
        proj8(pkrs[0:64, 0:n], (lambda j, kc: wkr_sw[:, kc, :]), seg, t0, n, krswkeys, pkrs)
        if seg == "lat":
            sq0, sq1 = rot(sqA, "sq"), rot(sqA, "sq")
            for cc, sq in ((0, sq0), (1, sq1)):
                P.op("act", (lambda e, cc=cc, sq=sq: e.activation(out=sq[:, 0:n], in_=pq[cc][:, 0:n], func=AF.Square)), r=[pq[cc]], w=[sq])
            pss = next_ps()
            P.op("pe", (lambda e, pss=pss, sq0=sq0: e.matmul(pss[:, 0:n], lhsT=ones_b[:, :], rhs=sq0[:, 0:n], start=True, stop=False)), r=[ones_b, sq0], w=[pss])
            P.op("pe", (lambda e, pss=pss, sq1=sq1: e.matmul(pss[:, 0:n], lhsT=ones_b[:, :], rhs=sq1[:, 0:n], start=False, stop=True)), r=[ones_b, sq1], w=[pss])
            rr = rot(rrep, "rr")
            rsqrt_rep(pss, n, 1, rr)
            for cc in range(2):
                P.op("dve", (lambda e, cc=cc, rr=rr: e.scalar_tensor_tensor(out=cqn[:, cc, 0:n], in0=pq[cc][:, 0:n], scalar=gcol[:, cc:cc + 1], in1=rr[:, 0:n], op0=ALU.mult, op1=ALU.mult)),
                     r=[pq[cc], rr] + gkeys, w=[(cqn, cc)])
        sqk = rot(sqA, "sq")
        P.op("act", (lambda e, sqk=sqk: e.activation(out=sqk[:, 0:n], in_=pkv[:, 0:n], func=AF.Square)), r=[pkv], w=[sqk])
        pss = next_ps()
        P.op("pe", (lambda e, pss=pss, sqk=sqk: e.matmul(pss[:, 0:n], lhsT=ones_b[:, :], rhs=sqk[:, 0:n], start=True, stop=True)), r=[ones_b, sqk], w=[pss])
        rr = rot(rrep, "rr")
        rsqrt_rep(pss, n, 2, rr)
        P.op("dve", (lambda e, rr=rr: e.scalar_tensor_tensor(out=ckvn[:, 0:n], in0=pkv[:, 0:n], scalar=gcol[:, 2:3], in1=rr[:, 0:n], op0=ALU.mult, op1=ALU.mult)),
             r=[pkv, rr] + gkeys, w=[ckvn])
        P.op("act", (lambda e: e.activation(out=krsq[:, 0:n], in_=pkr[0:64, 0:n], func=AF.Square)), r=[pkr], w=[krsq])
        for h in range(4):
            pkn = next_ps()
            P.op("pe", (lambda e, pkn=pkn, h=h: e.matmul(pkn[:, 0:n], lhsT=w_ukv_b[:, h * 256:h * 256 + 128], rhs=ckvn[:, 0:n], start=True, stop=True)),
                 r=[w_ukv_b, ckvn], w=[pkn])
            head_prep(pkn, pkr, pkrs, krsq, 6, 7, 8, n, tg0, KnT_d[h, :, tg0:tg0 + n], KrT_d[h, :, tg0:tg0 + n])
        for i in range(ntile):
            pv = next_ps()
            P.op("pe", (lambda e, pv=pv, i=i: e.matmul(pv[:].rearrange("p (h c) -> p h c", c=128), lhsT=ckvn[:, i * 128:(i + 1) * 128],
                                                       rhs=w_ukv_b[:].rearrange("p (h c) -> p h c", c=256)[:, :, 128:256], start=True, stop=True)),
                 r=[w_ukv_b, ckvn], w=[pv])
            tb = rot(tokb, "tok")
            P.op("act", (lambda e, pv=pv, tb=tb: e.activation(out=tb[:], in_=pv[:], func=AF.Copy)), r=[pv], w=[tb])
            P.dma("sp", Vt_d[tg0 + i * 128:tg0 + (i + 1) * 128, :], tb[:], r=[tb], w=["mla_scr"], nowaw=True)
        if seg == "lat":
            for h in range(4):
                pqn = PS[0]
                for cc in range(2):
                    P.op("pe", (lambda e, pqn=pqn, h=h, cc=cc: e.matmul(pqn[:, 0:n], lhsT=w_uq_b[:, cc, h * 192:h * 192 + 128], rhs=cqn[:, cc, 0:n], start=(cc == 0), stop=(cc == 1))),
                         r=[w_uq_b, (cqn, 0), (cqn, 1)], w=[pqn])
                pqr = PS[1]
                for cc in range(2):
                    P.op("pe", (lambda e, pqr=pqr, h=h, cc=cc: e.matmul(pqr[0:64, 0:n], lhsT=w_uq_b[:, cc, h * 192 + 128:h * 192 + 192], rhs=cqn[:, cc, 0:n], start=(cc == 0), stop=(cc == 1))),
                         r=[w_uq_b, (cqn, 0), (cqn, 1)], w=[pqr])
                pqrs = PS[2]
                for cc in range(2):
                    P.op("pe", (lambda e, pqrs=pqrs, h=h, cc=cc: e.matmul(pqrs[0:64, 0:n], lhsT=w_uq_sw[:, cc, h, :], rhs=cqn[:, cc, 0:n], start=(cc == 0), stop=(cc == 1))),
                         r=uqswkeys + [(cqn, 0), (cqn, 1)], w=[pqrs])
                qsq = rot(sqA, "sq")
                P.op("act", (lambda e, qsq=qsq, pqr=pqr: e.activation(out=qsq[0:64, 0:n], in_=pqr[0:64, 0:n], func=AF.Square)), r=[pqr], w=[qsq])
                head_prep(pqn, pqr, pqrs, qsq, 3, 4, 5, n, tg0, QnT_d[h, :, t0:t0 + n], QrT_d[h, :, t0:t0 + n])
        for cc in range(4):
            pf = next_ps()
            proj8(pf[:, 0:n], (lambda j, kc, cc=cc: wtap[:, j, kc, cc * 128:(cc + 1) * 128]), seg, t0, n, tapkeys, pf, taps=True)
            fm = rot(fmT, "fm")
            P.op("act", (lambda e, pf=pf, fm=fm: e.activation(out=fm[:, 0:n], in_=pf[:, 0:n], func=AF.Silu)), r=[pf], w=[fm])
            if cc < 2:
                P.op("pool", (lambda e, fm=fm: e.tensor_scalar(out=fm[:, 0:n], in0=fm[:, 0:n], scalar1=0.125, scalar2=None, op0=ALU.mult)), r=[fm], w=[fm])
                for hh in range(2):
                    P.dma("sp", mqT_d[2 * cc + hh, :, tg0:tg0 + n], fm[hh * 64:(hh + 1) * 64, 0:n], r=[fm], w=["ml_scr"], nowaw=True)
            else:
                for hh in range(2):
                    P.dma("sp", mkT_d[2 * (cc - 2) + hh, :, tg0:tg0 + n], fm[hh * 64:(hh + 1) * 64, 0:n], r=[fm], w=["ml_scr"], nowaw=True)
        for i in range(ntile):
            tt = t0 + i * 128
            tg = tg0 + i * 128
            pk = next_ps()
            k = 0
            for j in range(3):
                for kc in range(8):
                    P.op("pe", (lambda e, pk=pk, j=j, kc=kc, k=k, tt=tt: e.matmul(pk[:, 0:256], lhsT=hcols(seg, tt, 128, kc, j - 1), rhs=wtap[:, j, kc, 256:512],
                                                                           start=(k == 0), stop=(k == 23))),
                         r=tapkeys + hkeys, w=[pk])
                    k += 1
            tb = rot(tokb, "tok")
            P.op("act", (lambda e, pk=pk, tb=tb: e.activation(out=tb[:, 0:256], in_=pk[:, 0:256], func=AF.Silu)), r=[pk], w=[tb])
            P.dma("sp", mk_d[tg:tg + 128, :], tb[:, 0:256], r=[tb], w=["ml_scr"], nowaw=True)
            pv = next_ps()
            for kc in range(8):
                P.op("pe", (lambda e, pv=pv, kc=kc, tt=tt: e.matmul(pv[:], lhsT=hcols(seg, tt, 128, kc), rhs=w_in_b[:, kc, ML_V0:ML_V0 + 512], start=(kc == 0), stop=(kc == 7))),
                     r=winkeys + hkeys, w=[pv])
            tb = rot(tokb, "tok")
            P.op("dve", (lambda e, pv=pv, tb=tb: e.tensor_copy(out=tb[:], in_=pv[:])), r=[pv], w=[tb])
            P.dma("sp", mv_d[tg:tg + 128, :], tb[:], r=[tb], w=["ml_scr"], nowaw=True)
            if seg == "lat":
                po = next_ps()
                for kc in range(8):
                    P.op("pe", (lambda e, po=po, kc=kc, tt=tt: e.matmul(po[:], lhsT=hcols(seg, tt, 128, kc), rhs=w_in_b[:, kc, ML_O0:ML_O0 + 512], start=(kc == 0), stop=(kc == 7))),
                         r=winkeys + hkeys, w=[po])
                tb = rot(tokb, "tok")
                P.op("act", (lambda e, po=po, tb=tb: e.activation(out=tb[:], in_=po[:], func=AF.Sigmoid)), r=[po], w=[tb])
                P.dma("sp", mo_d[tt:tt + 128, :], tb[:], r=[tb], w=["ml_scr"], nowaw=True)
            pg = next_ps()
            for kc in range(8):
                P.op("pe", (lambda e, pg=pg, kc=kc, tt=tt: e.matmul(pg[:, 0:16], lhsT=hcols(seg, tt, 128, kc), rhs=w_in_b[:, kc, ML_G0:ML_G0 + 16], start=(kc == 0), stop=False)),
                     r=winkeys + hkeys, w=[pg])
            P.op("pe", (lambda e, pg=pg: e.matmul(pg[:, 0:16], lhsT=ones_f[0:1, :], rhs=brow16[0:1, :], start=False, stop=True)), r=[ones_f, (brow16, 0), (brow16, 1)], w=[pg])
            gt = rot(gat, "gat")
            P.op("dve", (lambda e, pg=pg, gt=gt: e.tensor_copy(out=gt[:], in_=pg[:, 0:16])), r=[pg], w=[gt])
            P.dma("sp", mg_d[tg:tg + 128, :], gt[:], r=[gt], w=["ml_scr"], nowaw=True)

    for bi, (seg, t0, n) in enumerate(blocks):
        block_body(bi, seg, t0, n)
    P.flush()
    esC.close()
    esH.close()
    if stop_after == "C":
        P.op("sp", lambda e: e.nop(), r=["mla_scr", "ml_scr"])
        P.flush(final_wait_keys=["mla_scr", "ml_scr"])
        es.close()
        return nc

    esD = ExitStack()

    def sbD(name, shape, dt):
        return esD.enter_context(nc.sbuf_tensor(name, list(shape), dt))

    maskf = sbD("maskf", [128, 128], F32)
    maskb = sbD("maskb", [128, 128], F32)
    Gt = sbD("Gt", [128, NTILE, 16], F32)
    nlf = sbD("nlf", [128, NTILE, 8], F32)
    ncf = sbD("ncf", [128, NTILE, 8], F32)
    nFt = sbD("nFt", [128, NTILE, 8], F32)
    colw = sbD("colw", [128, NTILE, 8], F32)
    flo = sbD("flo", [128, NTILE, 8], F32)
    dec = sbD("dec", [128, NTILE, 8], F32)
    gmlrep = sbD("gmlrep", [128, 512], F32)
    hbuf = sbD("hbuf", [128, 32, 4, 128], F32)
    Cst = [sbD("Cst%d" % d, [64, 4, 132], F32) for d in range(2)]
    P.dma("sp", maskf[:], maskf_d, w=[maskf], semkey="misc_ld", nowaw=True)
    P.dma("sp", maskb[:], maskb_d, w=[maskb], semkey="misc_ld", nowaw=True)
    P.dma("sp", gmlrep[:], g_ml_d.rearrange("(o n) -> o n", o=1).to_broadcast([128, 512]), w=[gmlrep], semkey="misc_ld", nowaw=True)
    P.dma("sp", Gt[:], mg_d.rearrange("(n p) c -> p n c", p=128), r=["ml_scr"], w=[Gt])
    for d in range(2):
        P.op("pool", (lambda e, d=d: e.memset(Cst[d][:], 0.0)), w=[(Cst[d], h) for h in range(4)])
    P.op("act", lambda e: e.activation(out=nlf[:], in_=Gt[:, :, 8:16], func=AF.Exp, scale=-1.0), r=[Gt], w=[nlf])
    P.op("act", lambda e: e.activation(out=nlf[:], in_=nlf[:], func=AF.Ln, bias=ones_f[:, 0:1], scale=1.0), r=[nlf, ones_f], w=[nlf])
    pcf = PS[0]
    nlf_s = sbD("nlf_s", [128, 2, NTILE, 4], F32)
    for d_ in range(2):
        P.op("dve", (lambda e, d_=d_: e.tensor_copy(out=nlf_s[:, d_, :, :], in_=nlf[:, :, d_ * 4:d_ * 4 + 4])), r=[nlf], w=[(nlf_s, d_)])
    P.op("pe", lambda e: e.matmul(pcf[:, 0:136], lhsT=maskf[:], rhs=nlf_s[:, 0, :, :].rearrange("p n h -> p (n h)"), start=True, stop=True), r=[maskf, (nlf_s, 0)], w=[pcf])
    P.op("pe", lambda e: e.matmul(pcf[:, 136:272], lhsT=maskb[:], rhs=nlf_s[:, 1, :, :].rearrange("p n h -> p (n h)"), start=True, stop=True), r=[maskb, (nlf_s, 1)], w=[pcf])
    P.op("dve", lambda e: e.tensor_copy(out=ncf[:, :, 0:4], in_=pcf[:, 0:136].rearrange("p (n h) -> p n h", h=4)), r=[pcf], w=[(ncf, 0)])
    P.op("dve", lambda e: e.tensor_copy(out=ncf[:, :, 4:8], in_=pcf[:, 136:272].rearrange("p (n h) -> p n h", h=4)), r=[pcf], w=[(ncf, 1)])
    pft = PS[1]
    P.op("pe", lambda e: e.matmul(pft[:, 0:272], lhsT=ones_f[:], rhs=nlf[:].rearrange("p n h -> p (n h)"), start=True, stop=True), r=[ones_f, nlf], w=[pft])
    P.op("dve", lambda e: e.tensor_copy(out=nFt[:].rearrange("p n h -> p (n h)"), in_=pft[:, 0:272]), r=[pft], w=[nFt])
    P.op("dve", lambda e: e.tensor_tensor(out=flo[:], in0=ncf[:], in1=nFt[:], op=ALU.subtract), r=[(ncf, 0), (ncf, 1), nFt], w=[flo])
    P.op("dve", lambda e: e.tensor_tensor(out=colw[:], in0=flo[:], in1=Gt[:, :, 0:8], op=ALU.add), r=[flo, Gt], w=[colw])
    P.op("act", lambda e: e.activation(out=flo[:], in_=flo[:], func=AF.Exp), r=[flo], w=[flo])
    P.op("act", lambda e: e.activation(out=colw[:], in_=colw[:], func=AF.Exp), r=[colw], w=[colw])
    P.op("act", lambda e: e.activation(out=dec[:], in_=nFt[:], func=AF.Exp, scale=-1.0), r=[nFt], w=[dec])

    if "colw_dbg" in debug:
        colw_dbg = dscratch("colw_dbg", [3, 128, NTILE, 8], F32)
        P.dma("sp", colw_dbg[0], colw[:], r=[colw], w=["colw_dbg"], nowaw=True)
        P.dma("sp", colw_dbg[1], flo[:], r=[flo], w=["colw_dbg"], nowaw=True)
        P.dma("sp", colw_dbg[2], dec[:], r=[dec], w=["colw_dbg"], nowaw=True)
    if stop_after == "D0":
        P.op("sp", lambda e: e.nop(), r=["colw_dbg"])
        P.flush(final_wait_keys=["colw_dbg"])
        esD.close()
        es.close()
        return nc
    NB = 2
    qTt = [[sbD("qTt%d_%d" % (d, i), [64, 4, 128], BF16) for i in range(NB)] for d in range(2)]
    kTt = [[sbD("kTt%d_%d" % (d, i), [64, 4, 128], BF16) for i in range(NB)] for d in range(2)]
    ktk = [[sbD("ktk%d_%d" % (d, i), [128, 256], BF16) for i in range(NB)] for d in range(2)]
    vau = [[sbD("vau%d_%d" % (d, i), [128, 4, 132], BF16) for i in range(NB)] for d in range(2)]
    vw = [[sbD("vw%d_%d" % (d, i), [128, 4, 132], BF16) for i in range(NB)] for d in range(2)]
    pT = [[sbD("pT%d_%d" % (d, i), [128, 4, 128], BF16) for i in range(NB)] for d in range(2)]
    Bbf = [[sbD("Bbf%d_%d" % (d, i), [64, 4, 132], BF16) for i in range(NB)] for d in range(2)]
    sgo = [sbD("sgo%d" % i, [128, 512], BF16) for i in range(2)]
    hs = [sbD("hs%d" % i, [128, 4, 128], F32) for i in range(2)]
    hjunk = sbD("hjunk", [128, 128], BF16)
    dd = [sbD("dd%d" % i, [128, 4], F32) for i in range(4)]
    ssn = [sbD("ssn%d" % i, [128, 4], F32) for i in range(2)]
    mixb = [sbD("mixb%d" % i, [128, 512], BF16) for i in range(2)]
    mixTs = [sbD("mixTs%d" % i, [128, 4, 128], BF16) for i in range(2)]
    for d in range(2):
        for i in range(NB):
            P.op("pool", (lambda e, d=d, i=i: e.memset(vau[d][i][:, :, 128:132], 0.0)), w=[(vau[d][i], "one")])
            P.op("pool", (lambda e, d=d, i=i: e.memset(vau[d][i][:, :, 128:129], 1.0)), r=[(vau[d][i], "one")], w=[(vau[d][i], "one")])
    ddc = [0]
    fin = [0]

    def order(d, j):
        if d == 0:
            return j
        return 1 - j if j < 2 else 35 - j

    def ml_load(d, j):
        g = order(d, j)
        i = j % NB
        tsl = slice(g * 128, (g + 1) * 128)
        sk = "mlld%d_%d" % (d, i)
        allw = [qTt[d][i], kTt[d][i], ktk[d][i], (vau[d][i], "v")]
        P.dma("sp", qTt[d][i][:], mqT_d[:, :, tsl].rearrange("h p t -> p h t"), r=["ml_scr"], w=allw, semkey=sk, nowaw=True)
        P.dma("sp", kTt[d][i][:], mkT_d[:, :, tsl].rearrange("h p t -> p h t"), r=["ml_scr"], w=allw, semkey=sk, nowaw=True)
        P.dma("act", ktk[d][i][:], mk_d[tsl, :], r=["ml_scr"], w=allw, semkey=sk, nowaw=True)
        P.dma("act", vau[d][i][:, :, 0:128], mv_d[tsl, :].rearrange("t (h c) -> t h c", c=128), r=["ml_scr"], w=allw, semkey=sk, nowaw=True)

    def ml_step(d, j):
        g = order(d, j)
        i = j % NB
        lat = g >= 2
        gl = g - 2
        QT, KT, KK, VA, VW, PT, BB = qTt[d][i], kTt[d][i], ktk[d][i], vau[d][i], vw[d][i], pT[d][i], Bbf[d][i]
        vakeys = [(VA, "one"), (VA, "v")]
        mask = maskf if d == 0 else maskb
        C = Cst[d]
        for h in range(4):
            P.op("dve", (lambda e, h=h: e.tensor_scalar(out=VW[:, h, :], in0=VA[:, h, :], scalar1=colw[:, g, d * 4 + h:d * 4 + h + 1], scalar2=None, op0=ALU.mult)),
                 r=vakeys + [colw], w=[(VW, h)])
        if lat:
            pS = PS[d]
            for h in range(4):
                c, po = h // 2, (h % 2) * 64
                P.op("pe", (lambda e, h=h, c=c, po=po: e.matmul(pS[:, h * 128:(h + 1) * 128], lhsT=KT[:, h, :], rhs=QT[:, h, :], start=True, stop=True)),
                     r=[KT, QT], w=[pS])
            for h in range(4):
                P.op("dve", (lambda e, h=h: e.scalar_tensor_tensor(out=PT[:, h, :], in0=pS[:, h * 128:(h + 1) * 128], scalar=colw[:, g, d * 4 + h:d * 4 + h + 1], in1=mask[:], op0=ALU.mult, op1=ALU.mult)),
                     r=[pS, colw, mask], w=[(PT, h)])
            for h in range(4):
                po = (h % 2) * 64
                P.op("act", (lambda e, h=h, po=po: e.activation(out=BB[:, h, :], in_=C[:, h, :], func=AF.Copy, scale=dec[0:64, g, d * 4 + h:d * 4 + h + 1])),
                     r=[(C, h), dec], w=[(BB, h)])
            pO = [PS[4], PS[5]]
            for h in range(4):
                c, po = h // 2, (h % 2) * 64
                ob = pO[h // 2][:, (h % 2) * 256:(h % 2) * 256 + 132]
                P.op("pe", (lambda e, h=h, ob=ob: e.matmul(ob, lhsT=PT[:, h, :], rhs=VA[:, h, :], start=True, stop=False)),
                     r=[(PT, h)] + vakeys, w=[pO[h // 2]])
                P.op("pe", (lambda e, h=h, ob=ob, c=c, po=po: e.matmul(ob, lhsT=QT[:, h, :], rhs=BB[:, h, :], start=False, stop=True)),
                     r=[QT, (BB, h)], w=[pO[h // 2]])
        pD = [PS[2], PS[3]]
        for h in range(4):
            c = h // 2
            ob = pD[c][0:64, (h % 2) * 256:(h % 2) * 256 + 132]
            P.op("pe", (lambda e, h=h, c=c, ob=ob: e.matmul(ob, lhsT=KK[:, h * 64:(h + 1) * 64], rhs=VW[:, h, :], start=True, stop=True)),
                 r=[KK, (VW, h)], w=[pD[c]])
        for h in range(4):
            c, po = h // 2, (h % 2) * 64
            P.op("dve", (lambda e, h=h, c=c, po=po: e.scalar_tensor_tensor(out=C[:, h, :], in0=C[:, h, :], scalar=dec[0:64, g, d * 4 + h:d * 4 + h + 1],
                                                                       in1=pD[c][0:64, (h % 2) * 256:(h % 2) * 256 + 132], op0=ALU.mult, op1=ALU.add)),
                 r=[(C, h), dec, pD[c]], w=[(C, h)])
        if not lat:
            return
        ddc[0] += 1
        DD = dd[ddc[0] % 4]
        for c in range(2):
            P.op("act", (lambda e, c=c: e.activation(out=DD[:, 2 * c:2 * c + 2], in_=pO[c][:, 0:512].rearrange("p (h n) -> p h n", n=256)[:, :, 128], func=AF.Abs)),
                 r=[pO[c]], w=[(DD, c)])
            P.op("dve", (lambda e, c=c: e.tensor_tensor(out=DD[:, 2 * c:2 * c + 2], in0=DD[:, 2 * c:2 * c + 2],
                                                        in1=flo[:, g, d * 4 + 2 * c:d * 4 + 2 * c + 2], op=ALU.max)),
                 r=[(DD, c), flo], w=[(DD, c)])
        P.op("dve", (lambda e: e.reciprocal(out=DD[:], in_=DD[:])), r=[(DD, 0), (DD, 1)], w=[DD])
        first = (g <= 17) == (d == 0)
        if first:
            for h in range(4):
                P.op("dve", (lambda e, h=h: e.tensor_scalar(out=hbuf[:, gl, h, :], in0=pO[h // 2][:, (h % 2) * 256:(h % 2) * 256 + 128], scalar1=DD[:, h:h + 1], scalar2=None, op0=ALU.mult)),
                     r=[pO[h // 2], DD], w=[(hbuf, gl, h)])
            return
        fin[0] += 1
        fi = fin[0] % 2
        HS, SSN, MB, MT, SG = hs[fi], ssn[fi], mixb[fi], mixTs[fi], sgo[fi]
        P.dma("act", SG[:], mo_d[gl * 128:(gl + 1) * 128, :], r=["ml_scr"], w=[SG])
        for h in range(4):
            P.op("dve", (lambda e, h=h: e.scalar_tensor_tensor(out=HS[:, h, :], in0=pO[h // 2][:, (h % 2) * 256:(h % 2) * 256 + 128], scalar=DD[:, h:h + 1], in1=hbuf[:, gl, h, :], op0=ALU.mult, op1=ALU.add)),
                 r=[pO[h // 2], DD, (hbuf, gl, h)], w=[(HS, h)])
            P.op("act", (lambda e, h=h: e.activation(out=hjunk[:], in_=HS[:, h, :], func=AF.Square, accum_out=SSN[:, h:h + 1])),
                 r=[(HS, h)], w=[hjunk, (SSN, h)])
        P.op("act", (lambda e: e.activation(out=SSN[:], in_=SSN[:], func=AF.Sqrt, bias=epsc[:, 0:1], scale=1.0 / 128)),
             r=[(SSN, h) for h in range(4)] + epskeys, w=[SSN])
        P.op("dve", (lambda e: e.reciprocal(out=SSN[:], in_=SSN[:])), r=[SSN], w=[SSN])
        for h in range(4):
            P.op("dve", (lambda e, h=h: e.scalar_tensor_tensor(out=HS[:, h, :], in0=HS[:, h, :], scalar=SSN[:, h:h + 1], in1=gmlrep[:, h * 128:(h + 1) * 128], op0=ALU.mult, op1=ALU.mult)),
                 r=[(HS, h), SSN, gmlrep], w=[(HS, h)])
        P.op("pool", (lambda e: e.tensor_tensor(out=MB[:], in0=HS[:].rearrange("p h c -> p (h c)"), in1=SG[:], op=ALU.mult)),
             r=[(HS, h) for h in range(4)] + [SG], w=[MB])
        pTr = PS[6 + fi]
        ptb = pTr[:].bitcast(BF16)
        for h in range(4):
            P.op("pe", (lambda e, h=h: e.transpose(out=ptb[:, h * 128:(h + 1) * 128], in_=MB[:, h * 128:(h + 1) * 128], identity=ident_b[:])),
                 r=[MB, ident_b], w=[pTr])
        P.op("act", (lambda e: e.activation(out=MT[:].rearrange("p h c -> p (h c)"), in_=ptb[:, 0:512], func=AF.Copy)), r=[pTr], w=[MT])
        P.dma("sp", mixT_d[4:8, :, gl * 128:(gl + 1) * 128].rearrange("h p t -> p h t"), MT[:], r=[MT], w=["mix_ml"], nowaw=True)

    for d in range(2):
        ml_load(d, 0)
    for j in range(min(NTILE, d_steps)):
        for d in range(2):
            if j + 1 < NTILE:
                ml_load(d, j + 1)
            ml_step(d, j)
    if "Cst_dbg" in debug:
        Cst_dbg = dscratch("Cst_dbg", [2, 64, 4, 132], F32)
        for d in range(2):
            P.dma("sp", Cst_dbg[d], Cst[d][:], r=[(Cst[d], h) for h in range(4)], w=["mix_ml"], nowaw=True)
    P.flush()
    esD.close()
    if stop_after == "D":
        P.op("sp", lambda e: e.nop(), r=["mix_ml"])
        P.flush(final_wait_keys=["mix_ml"])
        es.close()
        return nc

    esE = ExitStack()

    def sbE(name, shape, dt):
        return esE.enter_context(nc.sbuf_tensor(name, list(shape), dt))

    KnT = sbE("KnT", [128, 4, NT], BF16)
    KrT = sbE("KrT", [64, 4, NT], BF16)
    Vsb = sbE("Vsb", [128, NTILE, 512], BF16)
    w_out_b = sbE("w_out_b", [128, 8, D], BF16)
    grow = sbE("grow", [1, 2, 192], F32)
    gmx = sbE("gmx", [1, 4], F32)
    negC = sbE("negC", [128, 1], F32)
    g2rep = sbE("g2rep", [128, D], F32)
    tmpE = sbE("tmpE", [128, D], F32)
    cols2 = sbE("cols2", [128, 2, 8], F32)
    for h in range(4):
        P.dma("sp", KnT[:, h, :], KnT_d[h], r=["mla_scr"], w=[(KnT, h)], semkey="KT_ld", nowaw=True)
        P.dma("act", KrT[:, h, :], KrT_d[h], r=["mla_scr"], w=[(KrT, h)], semkey="KT_ld", nowaw=True)
    kkeys = [(KnT, h) for h in range(4)] + [(KrT, h) for h in range(4)]
    P.dma("sp", Vsb[:], Vt_d.rearrange("(n p) c -> p n c", p=128), r=["mla_scr"], w=[Vsb])
    w_out_v = w_out_d.rearrange("(k p) n -> p k n", p=128)
    for kc in range(8):
        P.dma("pool", w_out_b[:, kc, :], w_out_v[:, kc, :], w=[(w_out_b, kc)], semkey="w_out_ld", nowaw=True)
    wokeys = [(w_out_b, kc) for kc in range(8)]
    P.dma("sp", grow[0:1, 0, :], g_qn_d.rearrange("(o n) -> o n", o=1), w=[(grow, 0)], semkey="misc_ld", nowaw=True)
    P.dma("sp", grow[0:1, 1, :], g_kn_d.rearrange("(o n) -> o n", o=1), w=[(grow, 1)], semkey="misc_ld", nowaw=True)
    P.dma("sp", g2rep[:], g2_d.rearrange("(o n) -> o n", o=1).to_broadcast([128, D]), w=[g2rep], semkey="misc_ld", nowaw=True)
    P.op("act", lambda e: e.activation(out=grow[:], in_=grow[:], func=AF.Abs), r=[(grow, 0), (grow, 1)], w=[grow])
    P.op("dve", lambda e: e.tensor_reduce(out=gmx[0:1, 0:2], in_=grow[:], axis=AX.X, op=ALU.max), r=[grow], w=[(gmx, 0)])
    P.op("dve", lambda e: e.tensor_tensor(out=gmx[0:1, 2:3], in0=gmx[0:1, 0:1], in1=gmx[0:1, 1:2], op=ALU.mult), r=[(gmx, 0)], w=[(gmx, 1)])
    P.op("dve", lambda e: e.tensor_scalar(out=gmx[0:1, 3:4], in0=gmx[0:1, 2:3], scalar1=-math.sqrt(192.0), scalar2=None, op0=ALU.mult), r=[(gmx, 1)], w=[(gmx, 2)])
    P.op("pe", lambda e: e.matmul(PS[7][:, 0:1], lhsT=ones_f[0:1, :], rhs=gmx[0:1, 3:4], start=True, stop=True), r=[ones_f, (gmx, 2)], w=[PS[7]])
    P.op("dve", lambda e: e.tensor_copy(out=negC[:], in_=PS[7][:, 0:1]), r=[PS[7]], w=[negC])
    modkeys = [(modrep, j) for j in range(12)]
    P.op("dve", lambda e: e.scalar_tensor_tensor(out=g2rep[:], in0=modrep[:, 4 * D:5 * D], scalar=1.0, in1=g2rep[:], op0=ALU.add, op1=ALU.mult),
         r=modkeys + [g2rep], w=[g2rep])
    diag_extract(cols2[:, 0, :], g2rep[:], [g2rep], (cols2, 0), tmpE)
    diag_extract(cols2[:, 1, :], modrep[:, 3 * D:4 * D], modkeys, (cols2, 1), tmpE)

    if stop_after == "E0":
        e0_dbg = dscratch("e0_dbg", [128, 16], F32)
        e1_dbg = dscratch("e1_dbg", [128, 1], F32)
        P.dma("sp", e1_dbg[:, :], negC[:], r=[negC], w=["e0_dbg"], nowaw=True)
        P.dma("sp", e0_dbg[:, :], cols2[:].rearrange("p a k -> p (a k)"), r=[(cols2, 0), (cols2, 1)], w=["e0_dbg"], nowaw=True)
        P.op("sp", lambda e: e.nop(), r=["e0_dbg"] + kkeys + [Vsb] + wokeys)
        P.flush(final_wait_keys=["e0_dbg"])
        esE.close()
        es.close()
        return nc
    Qn = [sbE("Qn%d" % i, [128, 512], BF16) for i in range(2)]
    Qr = [sbE("Qr%d" % i, [64, 512], BF16) for i in range(2)]
    PTt = [sbE("PTt%d" % i, [128, 512], BF16) for i in range(3)]
    rden = sbE("rden", [128, 512], F32)
    mixa = [sbE("mixa%d" % i, [128, 4, 512], BF16) for i in range(1)] * 2
    mixm = [sbE("mixm%d" % i, [128, 4, 512], BF16) for i in range(1)] * 2
    xE = [sbE("xE%d" % i, [128, D], F32) for i in range(1)] * 2
    x1blk = sbE("x1blk", [128, 4, D], F32)
    xsblk = sbE("xsblk", [128, 4, D], BF16)
    h2blk = sbE("h2blk", [128, 8, 512], BF16)
    ssblk = sbE("ssblk", [128, 8], F32)
    ecnt = {"q": 0, "pt": 0, "t": 0}

    def attn_block(qb):
        q0 = qb * 512
        MA = mixa[qb % 2]
        MM = mixm[qb % 2]
        P.dma("act", MM[:], mixT_d[4:8, :, q0:q0 + 512].rearrange("h p t -> p h t"), r=["mix_ml"], w=[MM])
        for h in range(4):
            ecnt["q"] += 1
            QN, QR = Qn[ecnt["q"] % 2], Qr[ecnt["q"] % 2]
            P.dma("sp", QN[:], QnT_d[h, :, q0:q0 + 512], r=["mla_scr"], w=[QN])
            P.dma("sp", QR[:], QrT_d[h, :, q0:q0 + 512], r=["mla_scr"], w=[QR])
            pO, pDn = PS[2 + (ecnt["q"] % 2)], PS[4 + (ecnt["q"] % 2)]
            for kt in range(NTILE):
                pS = PS[kt % 2]
                ks = slice(kt * 128, (kt + 1) * 128)
                P.op("pe", (lambda e, pS=pS, ks=ks, h=h, QN=QN: e.matmul(pS[:], lhsT=KnT[:, h, ks], rhs=QN[:], start=True, stop=False)), r=[(KnT, h), QN], w=[pS])
                P.op("pe", (lambda e, pS=pS, ks=ks, h=h, QR=QR: e.matmul(pS[:], lhsT=KrT[:, h, ks], rhs=QR[:], start=False, stop=True)), r=[(KrT, h), QR], w=[pS])
                if elevel < 1:
                    continue
                ecnt["pt"] += 1
                PT = PTt[ecnt["pt"] % 3]
                P.op("act", (lambda e, pS=pS, PT=PT: e.activation(out=PT[:], in_=pS[:], func=AF.Exp, bias=negC[:], scale=1.0)), r=[pS, negC], w=[PT])
                if elevel < 2:
                    continue
                P.op("pe", (lambda e, PT=PT, kt=kt, h=h, pO=pO: e.matmul(pO[:], lhsT=Vsb[:, kt, h * 128:(h + 1) * 128], rhs=PT[:], start=(kt == 0), stop=(kt == NTILE - 1))),
                     r=[Vsb, PT], w=[pO])
                P.op("pe", (lambda e, PT=PT, kt=kt, pDn=pDn: e.matmul(pDn[:], lhsT=ones_b[:], rhs=PT[:], start=(kt == 0), stop=(kt == NTILE - 1))),
                     r=[ones_b, PT], w=[pDn])
            if elevel < 3:
                continue
            P.op("dve", (lambda e, pDn=pDn: e.reciprocal(out=rden[:], in_=pDn[:])), r=[pDn], w=[rden], name="rdenop")
            P.op("dve", (lambda e, pO=pO, h=h: e.scalar_tensor_tensor(out=MA[:, h, :], in0=pO[:], scalar=1.0, in1=rden[:], op0=ALU.mult, op1=ALU.mult)), r=[pO, rden], w=[(MA, h)], name="MAop")
        if "mixa_dbg" in debug:
            for h in range(4):
                P.op("act", (lambda e, h=h: e.activation(out=PTt[h % 3][:], in_=MA[:, h, :], func=AF.Copy)), r=[(MA, h)], w=[PTt[h % 3]], name="MAcopy")
                P.dma("sp", mixT_d[h, :, q0:q0 + 512], PTt[h % 3][:], r=[PTt[h % 3]], w=["x1_scr"], nowaw=True)
        X1B, XSB, H2B, SSB = x1blk, xsblk, h2blk, ssblk

        def out_tile(i):
            ecnt["t"] += 1
            ti = ecnt["t"] % 2
            tt = q0 + i * 128
            XE = xE[ti]
            P.dma("act", XE[:], x_d[tt:tt + 128, :], w=[XE])
            pY = [PS[6], PS[7]]
            for half in range(2):
                for kc in range(8):
                    src = MA if kc < 4 else MM
                    P.op("pe", (lambda e, half=half, kc=kc, src=src, i=i: e.matmul(pY[half][:], lhsT=src[:, kc % 4, i * 128:(i + 1) * 128], rhs=w_out_b[:, kc, half * 512:(half + 1) * 512],
                                                                              start=(kc == 0), stop=(kc == 7))),
                         r=[(MA, h) for h in range(4)] + [MM] + wokeys, w=[pY[half]])
            for half in range(2):
                hs_ = slice(half * 512, (half + 1) * 512)
                P.op("dve", (lambda e, half=half, hs_=hs_: e.scalar_tensor_tensor(out=X1B[:, i, hs_], in0=pY[half][:], scalar=1.0, in1=modrep[:, 2 * D + half * 512:2 * D + (half + 1) * 512], op0=ALU.mult, op1=ALU.mult)),
                     r=[pY[half]] + modkeys, w=[(X1B, i, half)])
                P.op("pool", (lambda e, hs_=hs_: e.tensor_tensor(out=X1B[:, i, hs_], in0=X1B[:, i, hs_], in1=XE[:, hs_], op=ALU.add)), r=[(X1B, i, half), XE], w=[(X1B, i, half)])
            x1keys = [(X1B, i, 0), (X1B, i, 1)]
            P.dma("sp", x1_d[tt:tt + 128, :], X1B[:, i, :], r=x1keys, w=["x1_scr"], nowaw=True)
            P.op("act", (lambda e: e.activation(out=tmpE[:].bitcast(BF16)[:, 0:D], in_=X1B[:, i, :], func=AF.Square, accum_out=SSB[:, i:i + 1])), r=x1keys, w=[tmpE, (SSB, i)])

        def norm_block():
            allx1 = [(X1B, i, hf) for i in range(4) for hf in range(2)]
            P.op("act", (lambda e: e.activation(out=SSB[:, 4:8], in_=SSB[:, 0:4], func=AF.Sqrt, bias=epsc[:, 0:1], scale=1.0 / D)), r=[(SSB, i) for i in range(4)] + epskeys, w=[(SSB, "r")])
            P.op("dve", (lambda e: e.reciprocal(out=SSB[:, 4:8], in_=SSB[:, 4:8])), r=[(SSB, "r")], w=[(SSB, "r")])
            for i in range(4):
                P.op("dve", (lambda e, i=i: e.tensor_scalar(out=XSB[:, i, :], in0=X1B[:, i, :], scalar1=SSB[:, 4 + i:5 + i], scalar2=None, op0=ALU.mult)),
                     r=allx1 + [(SSB, "r")], w=[(XSB, i)])
            for kc in range(8):
                pt = PS[kc % 2]
                ptb = pt[:].bitcast(BF16)
                for i in range(4):
                    P.op("pe", (lambda e, ptb=ptb, i=i, kc=kc: e.transpose(out=ptb[:, i * 128:(i + 1) * 128], in_=XSB[:, i, kc * 128:(kc + 1) * 128], identity=ident_b[:])),
                         r=[(XSB, i), ident_b], w=[pt])
                if kc % 2 == 0:
                    P.op("act", (lambda e, ptb=ptb, kc=kc: e.activation(out=H2B[:, kc, :], in_=ptb[:, 0:512], func=AF.Identity,
                                                                      scale=cols2[:, 0, kc:kc + 1], bias=cols2[:, 1, kc:kc + 1])),
                         r=[pt, (cols2, 0), (cols2, 1)], w=[(H2B, kc)])
                else:
                    P.op("dve", (lambda e, ptb=ptb, kc=kc: e.tensor_scalar(out=H2B[:, kc, :], in0=ptb[:, 0:512],
                                                                         scalar1=cols2[:, 0, kc:kc + 1], scalar2=cols2[:, 1, kc:kc + 1], op0=ALU.mult, op1=ALU.add)),
                         r=[pt, (cols2, 0), (cols2, 1)], w=[(H2B, kc)])
            for kc in range(8):
                P.dma("sp", h2T_d[kc, :, q0:q0 + 512], H2B[:, kc, :], r=[(H2B, kc)], w=["x1_scr"], nowaw=True)

        if "skip_tiles" not in debug:
            for i in range(4):
                out_tile(i)
            if elevel >= 6:
                norm_block()

    for qb in range(e_blocks):
        attn_block(qb)
    P.flush()
    esE.close()
    if stop_after == "E":
        P.op("sp", lambda e: e.nop(), r=["x1_scr"])
        P.flush(final_wait_keys=["x1_scr"])
        es.close()
        return nc

    esF = ExitStack()

    def sbF(name, shape, dt):
        return esF.enter_context(nc.sbuf_tensor(name, list(shape), dt))

    NEG = -1.0e30
    w_pq_b = sbF("w_pq_b", [128, 8, 2048], BF16)
    Ksf = sbF("Ksf", [128, 16, 128], F32)
    KsT = sbF("KsT", [128, 16, 128], BF16)
    iota_f = sbF("iota_f", [128, 128], F32)
    thr = sbF("thr", [128, 16], F32)
    h2g = sbF("h2g", [128, 8, 512], BF16)
    qTs = sbF("qTs", [128, 16, 512], BF16)
    Ssc = sbF("Ssc", [128, 16, 128], F32)
    sv = sbF("sv", [128, 16, 16], F32)
    siu = sbF("siu", [128, 16, 16], U32)
    sif = sbF("sif", [128, 16, 16], F32)
    cand = sbF("cand", [128, 8, 256], F32)
    best = sbF("best", [128, 8, 16], F32)
    posu = sbF("posu", [128, 8, 16], U32)
    posf = sbF("posf", [128, 8, 16], F32)
    ak = sbF("ak", [128, 8, 16], F32)
    bk = sbF("bk", [128, 8, 16], F32)
    big = sbF("big", [128, 8, 16, 16], F32)
    sel3 = sbF("sel3", [128, 3, 128], F32)
    zs = sbF("zs", [128, 8], F32)
    T3 = sbF("T3", [128, 3, 128], F32)
    Roh2 = [sbF("Roh%d" % i, [128, 64, 128], BF16) for i in range(1)] * 2
    Loh2 = [sbF("Loh%d" % i, [128, 64, 128], BF16) for i in range(1)] * 2
    Wsb = [sbF("Wsb%d" % i, [128, 128, 64], BF16) for i in range(2)]
    w_pq_v = w_pq_d.rearrange("(k p) n -> p k n", p=128)
    for kc in range(8):
        P.dma("pool", w_pq_b[:, kc, :], w_pq_v[:, kc, :], w=[(w_pq_b, kc)], semkey="w_pq_ld", nowaw=True)
    wpqkeys = [(w_pq_b, kc) for kc in range(8)]
    P.dma("sp", Ksf[:], subk_d.rearrange("g k d -> k g d"), w=[Ksf], semkey="misc_ld", nowaw=True)
    P.dma("sp", iota_f[:], iota_d, w=[iota_f], semkey="misc_ld", nowaw=True)
    P.op("dve", lambda e: e.tensor_scalar(out=thr[:, 0:15], in0=iota_f[:, 1:16], scalar1=16.0, scalar2=None, op0=ALU.mult), r=[iota_f], w=[thr])
    for g4 in range(4):
        pt = PS[g4]
        for i in range(4):
            P.op("pe", (lambda e, pt=pt, i=i, g4=g4: e.transpose(out=pt[:, i * 128:(i + 1) * 128], in_=Ksf[:, g4 * 4 + i, :], identity=ident_f[:])), r=[Ksf, ident_f], w=[pt])
        P.op("act", (lambda e, pt=pt, g4=g4: e.activation(out=KsT[:, g4 * 4:g4 * 4 + 4, :].rearrange("p g k -> p (g k)"), in_=pt[:], func=AF.Copy)), r=[pt], w=[(KsT, g4)])
    kstkeys = [(KsT, g4) for g4 in range(4)]
    fcnt = {"w": 0}

    def route_group(gq):
        t0g = gq * 512
        P.dma("sp", h2g[:], h2T_d[:, :, t0g:t0g + 512].rearrange("k p t -> p k t"), r=["x1_scr"], w=[h2g])
        for hp in range(16):
            pt = PS[hp % 2]
            for kc in range(8):
                P.op("pe", (lambda e, pt=pt, hp=hp, kc=kc: e.matmul(pt[:], lhsT=w_pq_b[:, kc, hp * 128:(hp + 1) * 128], rhs=h2g[:, kc, :], start=(kc == 0), stop=(kc == 7))),
                     r=wpqkeys + [h2g], w=[pt])
            if hp % 2 == 0:
                P.op("act", (lambda e, pt=pt, hp=hp: e.activation(out=qTs[:, hp, :], in_=pt[:], func=AF.Copy)), r=[pt], w=[(qTs, hp)])
            else:
                P.op("dve", (lambda e, pt=pt, hp=hp: e.tensor_copy(out=qTs[:, hp, :], in_=pt[:])), r=[pt], w=[(qTs, hp)])
        for tl in range(4):
            route_tile(gq * 4 + tl, tl)

    def route_tile(tile, tl):
        qkeys = [(qTs, hp) for hp in range(16)]
        for g4 in range(4):
            pt = PS[2 + g4]
            for i in range(4):
                hp = g4 * 4 + i
                P.op("pe", (lambda e, pt=pt, i=i, hp=hp: e.matmul(pt[:, i * 128:(i + 1) * 128], lhsT=qTs[:, hp, tl * 128:(tl + 1) * 128], rhs=KsT[:, hp, :], start=True, stop=True)),
                     r=qkeys + kstkeys, w=[pt])
            if g4 % 2 == 0:
                P.op("act", (lambda e, pt=pt, g4=g4: e.activation(out=Ssc[:, g4 * 4:g4 * 4 + 4, :].rearrange("p g k -> p (g k)"), in_=pt[:], func=AF.Copy)), r=[pt], w=[(Ssc, g4)])
            else:
                P.op("dve", (lambda e, pt=pt, g4=g4: e.tensor_copy(out=Ssc[:, g4 * 4:g4 * 4 + 4, :].rearrange("p g k -> p (g k)"), in_=pt[:])), r=[pt], w=[(Ssc, g4)])
        for hp in range(16):
            k_ = (Ssc, hp // 4)
            for rnd in range(2):
                sl = slice(rnd * 8, rnd * 8 + 8)
                P.op("dve", (lambda e, hp=hp, sl=sl: e.max(out=sv[:, hp, sl], in_=Ssc[:, hp, :])), r=[k_], w=[(sv, hp)])
                P.op("dve", (lambda e, hp=hp, sl=sl: e.max_index(out=siu[:, hp, sl], in_max=sv[:, hp, sl], in_values=Ssc[:, hp, :])), r=[k_, (sv, hp)], w=[(siu, hp)])
                if rnd == 0:
                    P.op("dve", (lambda e, hp=hp, sl=sl: e.match_replace(out=Ssc[:, hp, :], in_to_replace=sv[:, hp, sl], in_values=Ssc[:, hp, :], imm_value=NEG)), r=[k_, (sv, hp)], w=[k_])
        svkeys = [(sv, hp) for hp in range(16)]
        sikeys = [(siu, hp) for hp in range(16)]
        P.op("dve", lambda e: e.tensor_copy(out=sif[:], in_=siu[:]), r=sikeys, w=[sif])
        svv = sv[:].rearrange("p (h q) a -> p h q a", q=2)
        sfv = sif[:].rearrange("p (h q) a -> p h q a", q=2)
        P.op("dve", lambda e: e.tensor_tensor(out=cand[:].rearrange("p h (a b) -> p h a b", b=16), in0=svv[:, :, 0, :].unsqueeze(3).to_broadcast([128, 8, 16, 16]),
                                              in1=svv[:, :, 1, :].unsqueeze(2).to_broadcast([128, 8, 16, 16]), op=ALU.add), r=svkeys, w=[cand])
        for h in range(8):
            for rnd in range(2):
                sl = slice(rnd * 8, rnd * 8 + 8)
                P.op("dve", (lambda e, h=h, sl=sl: e.max(out=best[:, h, sl], in_=cand[:, h, :])), r=[cand], w=[(best, h)])
                P.op("dve", (lambda e, h=h, sl=sl: e.max_index(out=posu[:, h, sl], in_max=best[:, h, sl], in_values=cand[:, h, :])), r=[cand, (best, h)], w=[(posu, h)])
                if rnd == 0:
                    P.op("dve", (lambda e, h=h, sl=sl: e.match_replace(out=cand[:, h, :], in_to_replace=best[:, h, sl], in_values=cand[:, h, :], imm_value=NEG)), r=[cand, (best, h)], w=[cand])
        bkeys = [(best, h) for h in range(8)]
        pkeys = [(posu, h) for h in range(8)]
        g3 = sel3[:, 2, :].rearrange("p (h k) -> p h k", k=16)
        P.op("dve", lambda e: e.tensor_tensor(out=g3, in0=best[:], in1=best[:, :, 0:1].to_broadcast([128, 8, 16]), op=ALU.subtract), r=bkeys, w=[(sel3, 2)])
        P.op("act", lambda e: e.activation(out=g3, in_=g3, func=AF.Exp), r=[(sel3, 2)], w=[(sel3, 2)])
        P.op("dve", lambda e: e.tensor_reduce(out=zs[:], in_=g3, axis=AX.X, op=ALU.add), r=[(sel3, 2)], w=[zs])
        P.op("dve", lambda e: e.reciprocal(out=zs[:], in_=zs[:]), r=[zs], w=[zs])
        P.op("dve", lambda e: e.tensor_tensor(out=g3, in0=g3, in1=zs[:].unsqueeze(2).to_broadcast([128, 8, 16]), op=ALU.mult), r=[(sel3, 2), zs], w=[(sel3, 2)])
        P.op("dve", lambda e: e.tensor_copy(out=posf[:], in_=posu[:]), r=pkeys, w=[posf])
        P.op("dve", lambda e: e.tensor_tensor(out=big[:, :, :, 0:15], in0=posf[:].unsqueeze(3).to_broadcast([128, 8, 16, 15]),
                                              in1=thr[:, 0:15].unsqueeze(1).unsqueeze(1).to_broadcast([128, 8, 16, 15]), op=ALU.is_ge), r=[posf, thr], w=[big])
        P.op("dve", lambda e: e.tensor_reduce(out=ak[:], in_=big[:, :, :, 0:15], axis=AX.X, op=ALU.add), r=[big], w=[ak])
        P.op("dve", lambda e: e.scalar_tensor_tensor(out=bk[:], in0=ak[:], scalar=-16.0, in1=posf[:], op0=ALU.mult, op1=ALU.add), r=[ak, posf], w=[bk])
        for (src, q, dsti) in ((ak, 0, 0), (bk, 1, 1)):
            P.op("dve", (lambda e, src=src: e.tensor_tensor(out=big[:], in0=src[:].unsqueeze(3).to_broadcast([128, 8, 16, 16]),
                                                           in1=iota_f[:, 0:16].unsqueeze(1).unsqueeze(1).to_broadcast([128, 8, 16, 16]), op=ALU.is_equal)), r=[src, iota_f, big], w=[big])
            P.op("dve", (lambda e, q=q: e.tensor_tensor(out=big[:], in0=big[:], in1=sfv[:, :, q, :].unsqueeze(2).to_broadcast([128, 8, 16, 16]), op=ALU.mult)), r=[big, sif], w=[big])
            P.op("dve", (lambda e, dsti=dsti: e.tensor_reduce(out=sel3[:, dsti, :].rearrange("p (h k) -> p h k", k=16), in_=big[:], axis=AX.X, op=ALU.add)), r=[big], w=[(sel3, dsti)])
        ptT = PS[6]
        for c3 in range(3):
            P.op("pe", (lambda e, c3=c3: e.transpose(out=ptT[:, c3 * 128:(c3 + 1) * 128], in_=sel3[:, c3, :], identity=ident_f[:])), r=[(sel3, c3), ident_f], w=[ptT])
        P.op("act", lambda e: e.activation(out=T3[:].rearrange("p c t -> p (c t)"), in_=ptT[:, 0:384], func=AF.Copy), r=[ptT], w=[T3])
        for half in range(2):
            hs_ = slice(half * 64, half * 64 + 64)
            Roh, Loh = Roh2[half], Loh2[half]
            onehot_half(half, hs_, Roh, Loh, tile)

    def onehot_half(half, hs_, Roh, Loh, tile):
        if True:
            P.op("dve", (lambda e, hs_=hs_: e.tensor_tensor(out=Roh[:], in0=iota_f[:].unsqueeze(1).to_broadcast([128, 64, 128]),
                                                           in1=T3[:, 0, hs_].unsqueeze(2).to_broadcast([128, 64, 128]), op=ALU.is_equal)), r=[T3, iota_f], w=[Roh])
            P.op("dve", (lambda e, hs_=hs_: e.tensor_tensor(out=Loh[:], in0=iota_f[:].unsqueeze(1).to_broadcast([128, 64, 128]),
                                                           in1=T3[:, 1, hs_].unsqueeze(2).to_broadcast([128, 64, 128]), op=ALU.is_equal)), r=[T3, iota_f], w=[Loh])
            P.op("dve", (lambda e, hs_=hs_: e.tensor_tensor(out=Loh[:], in0=Loh[:], in1=T3[:, 2, hs_].unsqueeze(2).to_broadcast([128, 64, 128]), op=ALU.mult)), r=[T3, Loh], w=[Loh])
            fcnt["w"] += 1
            WS = Wsb[fcnt["w"] % 2]
            for t4 in range(16):
                pw = PS[t4 % 2]
                for u in range(4):
                    t = t4 * 4 + u
                    P.op("pe", (lambda e, pw=pw, u=u, t=t: e.matmul(pw[:, u * 128:(u + 1) * 128], lhsT=Loh[:, t, :], rhs=Roh[:, t, :], start=True, stop=True)), r=[Loh, Roh], w=[pw])
                outv = WS[:, :, t4 * 4:t4 * 4 + 4].rearrange("j i t -> j t i")
                inv = pw[:].rearrange("j (t i) -> j t i", i=128)
                if t4 % 4 != 3:
                    P.op("act", (lambda e, outv=outv, inv=inv: e.activation(out=outv, in_=inv, func=AF.Copy)), r=[pw], w=[(WS, t4)])
                else:
                    P.op("dve", (lambda e, outv=outv, inv=inv: e.tensor_copy(out=outv, in_=inv)), r=[pw], w=[(WS, t4)])
            P.dma("sp", W_d[tile, half], WS[:], r=[(WS, t4) for t4 in range(16)], w=["W_scr"], nowaw=True)

    for gq in range(f_groups):
        route_group(gq)
    P.flush()
    esF.close()
    if stop_after == "F":
        P.op("sp", lambda e: e.nop(), r=["W_scr"])
        P.flush(final_wait_keys=["W_scr"])
        es.close()
        return nc

    esG = ExitStack()

    def sbG(name, shape, dt):
        return esG.enter_context(nc.sbuf_tensor(name, list(shape), dt))

    Uc = [sbG("Uc%d" % i, [128, D], BF16) for i in range(2)]
    UTs = [sbG("UTs%d" % i, [128, 8, 1024], BF16) for i in range(2)]
    Vs = [sbG("Vs%d" % i, [128, 8, D], BF16) for i in range(2)]
    Wsl = [sbG("Wsl%d" % i, [128, 8, 8, 128], BF16) for i in range(2)]
    h2sg = sbG("h2sg", [128, 8, 1024], BF16)
    acc = sbG("acc", [128, 8, D], F32)
    Gt_ = [sbG("Gt%d" % i, [128, 256], F32) for i in range(2)]
    PTg = [sbG("PTg%d" % i, [128, 256], BF16) for i in range(2)]
    x1g = sbG("x1g", [128, D], F32)
    og = sbG("og", [128, D], F32)
    gcnt = {"u": 0, "a": 0}
    modkeys = [(modrep, j) for j in range(12)]

    def expert_super(sg, sc):
        UT, V, W = UTs[sc % 2], Vs[sc % 2], Wsl[sc % 2]
        P.dma("pool", V[:], ev_d[sc * 1024:(sc + 1) * 1024, :].rearrange("(c p) d -> p c d", p=128), w=[V])
        for tile in range(8):
            for half in range(2):
                P.dma("sp" if half == 0 else "act", W[:, :, tile, half * 64:(half + 1) * 64], W_d[sg * 8 + tile, half, :, sc * 8:(sc + 1) * 8, :],
                      r=["W_scr"], w=[(W, tile, half)], semkey=(W, "ld"), nowaw=True)
        wkeys = [(W, tile, half) for tile in range(8) for half in range(2)]
        utkeys = [(UT, ci, q) for ci in range(8) for q in range(2)]
        if sg > 0:
            P.dma("sp", UT[:], UT_d[sc], r=["UT_scr"], w=utkeys, semkey=(UT, "ld"))
        for ci in (range(8) if sg == 0 else []):
            c = sc * 8 + ci
            gcnt["u"] += 1
            U_ = Uc[gcnt["u"] % 2]
            P.dma("pool", U_[:], eu_d[c * 128:(c + 1) * 128, :], w=[U_])
            for q in range(2):
                pt = PS[6 + q]
                ptb = pt[:].bitcast(BF16)
                for k4 in range(4):
                    kc = q * 4 + k4
                    P.op("pe", (lambda e, ptb=ptb, k4=k4, kc=kc, U_=U_: e.transpose(out=ptb[:, k4 * 128:(k4 + 1) * 128], in_=U_[:, kc * 128:(kc + 1) * 128], identity=ident_b[:])),
                         r=[U_, ident_b], w=[pt])
                outv = UT[:, q * 4:q * 4 + 4, ci * 128:(ci + 1) * 128]
                inv = ptb[:, 0:512].rearrange("p (k e) -> p k e", e=128)
                if q == 0:
                    P.op("act", (lambda e, outv=outv, inv=inv: e.activation(out=outv, in_=inv, func=AF.Copy)), r=[pt], w=[(UT, ci, q)])
                else:
                    P.op("dve", (lambda e, outv=outv, inv=inv: e.tensor_copy(out=outv, in_=inv)), r=[pt], w=[(UT, ci, q)])
        if sg == 0 and g_sgs > 1:
            P.dma("act", UT_d[sc], UT[:], r=utkeys, w=["UT_scr"], nowaw=True)
        for tp in range(4):
            for ci in range(8):
                gcnt["a"] += 1
                ai = gcnt["a"] % 2
                pA = PS[4 + ai]
                G_, PT_ = Gt_[ai], PTg[ai]
                for kc in range(8):
                    P.op("pe", (lambda e, pA=pA, kc=kc, ci=ci, tp=tp: e.matmul(pA[:, 0:256], lhsT=UT[:, kc, ci * 128:(ci + 1) * 128], rhs=h2sg[:, kc, tp * 256:(tp + 1) * 256],
                                                                         start=(kc == 0), stop=(kc == 7))),
                         r=[(UT, ci, 0), (UT, ci, 1), h2sg], w=[pA])
                P.op("act", (lambda e, pA=pA, G_=G_: e.activation(out=G_[:], in_=pA[:, 0:256], func=AF.Gelu)), r=[pA], w=[G_])
                P.op("dve", (lambda e, G_=G_, PT_=PT_, ci=ci, tp=tp: e.tensor_tensor(out=PT_[:], in0=G_[:], in1=W[:, ci, 2 * tp:2 * tp + 2, :].rearrange("p a t -> p (a t)"), op=ALU.mult)),
                     r=[G_] + wkeys, w=[PT_])
                for tl in range(2):
                    for h2_ in range(2):
                        pc = PS[tl * 2 + h2_]
                        P.op("pe", (lambda e, pc=pc, PT_=PT_, tl=tl, h2_=h2_, ci=ci: e.matmul(pc[:], lhsT=PT_[:, tl * 128:(tl + 1) * 128], rhs=V[:, ci, h2_ * 512:(h2_ + 1) * 512],
                                                                                     start=(ci == 0), stop=(ci == 7))),
                             r=[PT_, V], w=[pc])
            for tl in range(2):
                tile = tp * 2 + tl
                for h2_ in range(2):
                    pc = PS[tl * 2 + h2_]
                    dst = acc[:, tile, h2_ * 512:(h2_ + 1) * 512]
                    if sc == 0:
                        P.op("act", (lambda e, pc=pc, dst=dst: e.activation(out=dst, in_=pc[:], func=AF.Copy)), r=[pc], w=[(acc, tile, h2_)])
                    else:
                        P.op("dve", (lambda e, pc=pc, dst=dst: e.scalar_tensor_tensor(out=dst, in0=pc[:], scalar=1.0, in1=dst, op0=ALU.mult, op1=ALU.add)),
                             r=[pc, (acc, tile, h2_)], w=[(acc, tile, h2_)])

    def final_tile(sg, tile):
        tt = sg * 1024 + tile * 128
        P.dma("sp", x1g[:], x1_d[tt:tt + 128, :], r=["x1_scr"], w=[x1g])
        P.op("dve", lambda e: e.tensor_tensor(out=og[:], in0=acc[:, tile, :], in1=modrep[:, 5 * D:6 * D], op=ALU.mult),
             r=[(acc, tile, 0), (acc, tile, 1)] + modkeys, w=[og])
        P.op("pool", lambda e: e.tensor_tensor(out=og[:], in0=og[:], in1=x1g[:], op=ALU.add), r=[og, x1g], w=[og])
        P.dma("sp", out_d[tt:tt + 128, :], og[:], r=[og], w=["out"], nowaw=True)

    for sg in range(g_sgs):
        P.dma("sp", h2sg[:], h2T_d[:, :, sg * 1024:(sg + 1) * 1024].rearrange("k p t -> p k t"), r=["x1_scr"], w=[h2sg])
        for sc in range(g_scs):
            expert_super(sg, sc)
        for tile in range(8):
            final_tile(sg, tile)
    P.op("sp", lambda e: e.nop(), r=["out"])
    P.flush(final_wait_keys=["out"])
    esG.close()
    es.close()
    return nc


def make_in_maps(inputs):
    consts = _host_consts()
    maps = []
    for b in range(8):
        m = {
            "x": np.ascontiguousarray(inputs["x"][b]),
            "ctx": np.ascontiguousarray(inputs["ctx"][b]),
            "c": np.ascontiguousarray(inputs["c"][b]),
            "c_ctx": np.ascontiguousarray(inputs["c_ctx"]),
            "w_ada": np.ascontiguousarray(inputs["w_ada"][0]),
            "b_ada": np.ascontiguousarray(inputs["b_ada"][0]),
            "g_norm1": np.ascontiguousarray(inputs["g_norm1"][0]),
            "w_in": np.ascontiguousarray(inputs["w_in"][0]),
            "g_cq": np.ascontiguousarray(inputs["g_cq"][0]),
            "w_uq": np.ascontiguousarray(inputs["w_uq"][0]),
            "g_ckv": np.ascontiguousarray(inputs["g_ckv"][0]),
            "w_ukv": np.ascontiguousarray(inputs["w_ukv"][0]),
            "g_qn": np.ascontiguousarray(inputs["g_qn"][0]),
            "g_kn": np.ascontiguousarray(inputs["g_kn"][0]),
            "conv_qk": np.ascontiguousarray(inputs["conv_qk"][0]),
            "b_igate": np.ascontiguousarray(inputs["b_igate"][0].reshape(8)),
            "b_fgate": np.ascontiguousarray(inputs["b_fgate"][0].reshape(8)),
            "g_mlstm": np.ascontiguousarray(inputs["g_mlstm"][0]),
            "w_out": np.ascontiguousarray(inputs["w_out"][0]),
            "w_pq": np.ascontiguousarray(inputs["w_pq"][0]),
            "sub_keys": np.ascontiguousarray(inputs["sub_keys"][0].reshape(16, 128, 128)),
            "expert_u": np.ascontiguousarray(inputs["expert_u"][0]),
            "expert_v": np.ascontiguousarray(inputs["expert_v"][0]),
            "iota": consts["iota"],
            "g_norm2": np.ascontiguousarray(inputs["g_norm2"][0]),
            "mask_f": consts["mask_f"],
            "mask_b": consts["mask_b"],
            "ident": consts["ident"],
            "cosT": consts["cosT"],
            "sinT": consts["sinT"],
        }
        maps.append(m)
    return maps


def kernel(**inputs):
    nc = build_program()
    in_maps = make_in_maps(inputs)
    res = run_bass_kernel_spmd(nc, in_maps, core_ids=list(range(8)))
    return np.stack([r["out"] for r in res.results], axis=0)
```

```python
import math
from contextlib import ExitStack

import numpy as np
import ml_dtypes
import concourse.bass as bass
import concourse.mybir as mybir
from concourse.bass_utils import run_bass_kernel_spmd

F32 = mybir.dt.float32
BF16 = mybir.dt.bfloat16
U32 = mybir.dt.uint32
I32 = mybir.dt.int32
ALU = mybir.AluOpType
AF = mybir.ActivationFunctionType
AX = mybir.AxisListType

D = 1024
SEQ = 4096
CTX = 256
NT = SEQ + CTX
NTILE = NT // 128
EPS = 1e-6
IN_COLS = 2000
CTX_OFF = 1
LAT_OFF = CTX_OFF + CTX + 1
TP = LAT_OFF + SEQ + 1

SEM_WRAP = 20000
DUMP = None


class _Op:
    __slots__ = ("eng", "fn", "deps", "signal", "tok", "is_dma", "semkey", "name", "emit_tok")

    def __init__(self, eng, fn, is_dma=False, semkey=None, name=""):
        self.eng = eng
        self.fn = fn
        self.deps = []
        self.signal = False
        self.tok = None
        self.is_dma = is_dma
        self.semkey = semkey
        self.name = name


class Prog:
    ENGS = ("pe", "act", "dve", "pool", "sp")

    def __init__(self, nc, es, same_engine_sync=("act", "dve", "pool")):
        self.nc = nc
        self.es = es
        self.ops = []
        self.state = {}
        self.same = set(same_engine_sync)
        self.sems = {}
        self.eng_count = {e: 0 for e in self.ENGS}
        self.dma_count = {}
        self.known = {e: {} for e in self.ENGS}
        self.nsem = 0
        self.last_dma = {}

    def sem(self, name):
        if name not in self.sems:
            self.sems[name] = self.es.enter_context(self.nc.semaphore("s%d" % self.nsem))
            self.nsem += 1
        return self.sems[name]

    def _cls(self, op):
        return ("dma", op.semkey) if op.is_dma else op.eng

    @classmethod
    def _k(cls, k):
        if isinstance(k, (str, int)):
            return k
        if isinstance(k, tuple):
            return tuple(cls._k(x) for x in k)
        return ("T", k.name)

    def _record(self, op, reads, writes, nowaw=False):
        reads = [self._k(k) for k in reads]
        writes = [self._k(k) for k in writes]
        deps = {}

        def add(p):
            if p is None:
                return
            deps[id(p)] = p

        for k in reads:
            st = self.state.get(k)
            if st is not None:
                add(st[0])
        for k in writes:
            st = self.state.get(k)
            if st is not None:
                add(st[0])
                for r in st[1].values():
                    add(r)
        for p in list(deps.values()):
            if p is op:
                continue
            if nowaw and p.is_dma and op.is_dma and p.semkey == op.semkey:
                continue
            if p.is_dma:
                p = self.last_dma.get(p.semkey, p)
            if (not p.is_dma) and (not op.is_dma) and p.eng == op.eng and p.eng not in self.same:
                continue
            if (not p.is_dma) and op.is_dma and p.eng == op.eng and p.eng not in self.same:
                pass
            op.deps.append(p)
            p.signal = True
        for k in reads:
            st = self.state.setdefault(k, [None, {}])
            st[1][self._cls(op)] = op
        for k in writes:
            st = self.state.get(k)
            if (nowaw and st is not None and st[0] is not None and st[0].is_dma and op.is_dma
                    and st[0].semkey == op.semkey):
                self.state[k] = [op, st[1]]
            else:
                self.state[k] = [op, {}]
        if op.is_dma:
            self.last_dma[op.semkey] = op
        self.ops.append(op)
        return op

    def op(self, eng, fn, r=(), w=(), name=""):
        return self._record(_Op(eng, fn, name=name), list(r), list(w))

    def dma(self, q, out, in_, r=(), w=(), semkey=None, nowaw=False, **kw):
        w = list(w)
        r = list(r)
        if semkey is None:
            semkey = w[0] if w else r[0]
        semkey = self._k(semkey)
        op = _Op(q, lambda e: e.dma_start(out=out, in_=in_, **kw), is_dma=True, semkey=("d", semkey))
        op.signal = True
        return self._record(op, r, w, nowaw=nowaw)

    def _assign_tokens(self, ops):
        for op in ops:
            if not op.signal:
                continue
            if op.is_dma:
                c = self.dma_count.get(op.semkey, 0) + 16
                self.dma_count[op.semkey] = c
                op.tok = (op.semkey, c, 16)
            else:
                c = self.eng_count[op.eng]
                self.eng_count[op.eng] = c + 1
                op.tok = ((op.eng, c // SEM_WRAP), c % SEM_WRAP + 1, 1)

    def flush(self, final_wait_keys=()):
        ops = self.ops
        self.ops = []
        finals = []
        for k in final_wait_keys:
            st = self.state.get(self._k(k))
            if st is not None and st[0] is not None:
                st[0].signal = True
                finals.append(st[0])
        per_eng = {e: [] for e in self.ENGS}
        for op in ops:
            per_eng[op.eng].append(op)
        for e in self.ENGS:
            for op in reversed(per_eng[e]):
                if not op.is_dma:
                    op.signal = True
                    break
        self._assign_tokens(ops)
        self._back = []
        for e in self.ENGS:
            last = None
            for op in reversed(per_eng[e]):
                if op.is_dma:
                    continue
                if op.tok is not None:
                    last = op.tok
                    op.emit_tok = True
                else:
                    op.tok = last
                    op.emit_tok = False
        nc = self.nc
        for op in ops:
            if op.tok is not None:
                self.sem(op.tok[0])

        def run(engname, eng):
            known = self.known[engname]
            for op in per_eng[engname]:
                for p in op.deps:
                    s, v, _ = p.tok
                    if known.get(s, 0) >= v:
                        continue
                    eng.wait_ge(self.sems[s], v)
                    known[s] = v
                if DUMP is not None:
                    DUMP.append((engname, op.name, [p.tok for p in op.deps], op.tok, (op.is_dma or op.emit_tok)))
                ins = op.fn(eng)
                if op.is_dma or op.emit_tok:
                    ins.then_inc(self.sems[op.tok[0]], op.tok[2])
            for e2 in self.ENGS:
                if e2 == engname:
                    continue
                for op2 in reversed(per_eng[e2]):
                    if (not op2.is_dma) and op2.tok is not None:
                        s2, v2, _ = op2.tok
                        if known.get(s2, 0) < v2:
                            eng.wait_ge(self.sems[s2], v2)
                            known[s2] = v2
                        break
            for sk, cnt_ in self.dma_count.items():
                if known.get(sk, 0) < cnt_:
                    eng.wait_ge(self.sems[sk], cnt_)
                    known[sk] = cnt_
            if engname == "sp":
                for p in finals:
                    s, v, _ = p.tok
                    if known.get(s, 0) < v:
                        eng.wait_ge(self.sems[s], v)
                        known[s] = v

        with nc.Block() as block:
            @block.tensor
            def _(e):
                run("pe", e)

            @block.scalar
            def _(e):
                run("act", e)

            @block.vector
            def _(e):
                run("dve", e)

            @block.gpsimd
            def _(e):
                run("pool", e)

            @block.sync
            def _(e):
                run("sp", e)


MLA_COLS = 448
C_Q0, C_KV0, C_KR0 = 0, 256, 384
ML_Q0, ML_K0, ML_V0, ML_O0, ML_G0 = 448, 704, 960, 1472, 1984


def _swap_idx():
    idx = np.arange(64)
    half = (idx % 32) // 16
    return np.where(half == 0, idx + 16, idx - 16)


def _host_consts():
    c = {}
    c["ident"] = np.eye(128, dtype=np.float32)
    inv = (10000.0 ** (-np.arange(16, dtype=np.float32) * (2.0 / 32))).astype(np.float32)
    t = np.arange(SEQ)
    row = (t // 64).astype(np.float32)
    col = (t % 64).astype(np.float32)
    cosT = np.ones((64, NT), np.float32)
    sinT = np.zeros((64, NT), np.float32)
    for d in range(64):
        pos = row if d < 32 else col
        half = (d % 32) // 16
        ang = (pos * inv[d % 16]).astype(np.float32)
        cosT[d, CTX:] = np.cos(ang)
        sinT[d, CTX:] = np.sin(ang) * (-1.0 if half == 0 else 1.0)
    c["cosT"] = cosT
    c["sinT"] = sinT
    s_i = np.arange(128)[:, None]
    t_i = np.arange(128)[None, :]
    c["mask_f"] = (s_i <= t_i).astype(np.float32)
    c["mask_b"] = (s_i >= t_i).astype(np.float32)
    c["iota"] = np.tile(np.arange(128, dtype=np.float32)[None, :], (128, 1))
    return c


def build_program(debug=None, stop_after=None, d_steps=NTILE, e_blocks=8, elevel=9, f_groups=8, g_sgs=4, g_scs=16):
    debug = debug or set()
    nc = bass.Bass("TRN2", target_bir_lowering=False)
    es = ExitStack()
    P = Prog(nc, es)

    def din(name, shape, dt=F32):
        return nc.dram_tensor(name, list(shape), dt, kind="ExternalInput").ap()

    def dscratch(name, shape, dt):
        kind = "ExternalOutput" if name in debug else "Internal"
        return nc.dram_tensor(name, list(shape), dt, kind=kind).ap()

    x_d = din("x", [SEQ, D])
    ctx_d = din("ctx", [CTX, D])
    c_d = din("c", [D])
    cctx_d = din("c_ctx", [D])
    w_ada_d = din("w_ada", [D, 6 * D])
    b_ada_d = din("b_ada", [6 * D])
    g1_d = din("g_norm1", [D])
    w_in_d = din("w_in", [D, IN_COLS])
    g_cq_d = din("g_cq", [256])
    w_uq_d = din("w_uq", [256, 768])
    g_ckv_d = din("g_ckv", [128])
    w_ukv_d = din("w_ukv", [128, 1024])
    g_qn_d = din("g_qn", [192])
    g_kn_d = din("g_kn", [192])
    conv_d = din("conv_qk", [3, 512])
    b_ig_d = din("b_igate", [8])
    b_fg_d = din("b_fgate", [8])
    g_ml_d = din("g_mlstm", [512])
    w_out_d = din("w_out", [D, D])
    w_pq_d = din("w_pq", [D, 2048])
    subk_d = din("sub_keys", [16, 128, 128])
    eu_d = din("expert_u", [16384, D])
    ev_d = din("expert_v", [16384, D])
    iota_d = din("iota", [128, 128])
    g2_d = din("g_norm2", [D])
    maskf_d = din("mask_f", [128, 128])
    maskb_d = din("mask_b", [128, 128])
    ident_d = din("ident", [128, 128])
    cosT_d = din("cosT", [64, NT])
    sinT_d = din("sinT", [64, NT])
    out_d = nc.dram_tensor("out", [SEQ, D], F32, kind="ExternalOutput").ap()

    KnT_d = dscratch("KnT_d", [4, 128, NT], BF16)
    KrT_d = dscratch("KrT_d", [4, 64, NT], BF16)
    Vt_d = dscratch("Vt_d", [NT, 512], BF16)
    QnT_d = dscratch("QnT_d", [4, 128, SEQ], BF16)
    QrT_d = dscratch("QrT_d", [4, 64, SEQ], BF16)
    mqT_d = dscratch("mqT_d", [4, 64, NT], BF16)
    mkT_d = dscratch("mkT_d", [4, 64, NT], BF16)
    mk_d = dscratch("mk_d", [NT, 256], BF16)
    mv_d = dscratch("mv_d", [NT, 512], BF16)
    mo_d = dscratch("mo_d", [SEQ, 512], BF16)
    mg_d = dscratch("mg_d", [NT, 16], F32)
    mixT_d = dscratch("mixT_d", [8, 128, SEQ], BF16)
    x1_d = out_d
    h2T_d = dscratch("h2T_d", [8, 128, SEQ], BF16)
    W_d = dscratch("W_d", [32, 2, 128, 128, 64], BF16)
    UT_d = dscratch("UT_d", [16, 128, 8, 1024], BF16)

    hT_dbg = dscratch("hT_dbg", [128, 8, TP], BF16) if "hT_dbg" in debug else None
    mod_dbg = dscratch("mod_dbg", [128, 6 * D], F32) if "mod_dbg" in debug else None

    def sb(name, shape, dt):
        return es.enter_context(nc.sbuf_tensor(name, list(shape), dt))

    def psum(name, shape, dt=F32):
        return es.enter_context(nc.psum_tensor(name, list(shape), dt))

    ident_f = sb("ident_f", [128, 128], F32)
    ident_b = sb("ident_b", [128, 128], BF16)
    ones_f = sb("ones_f", [128, 128], F32)
    ones_b = sb("ones_b", [128, 128], BF16)
    epsc = sb("epsc", [128, 4], F32)
    modrep = sb("modrep", [128, 6 * D], F32)
    cols1 = sb("cols1", [128, 4, 8], F32)
    PS = [psum("ps%d" % i, [128, 512], F32) for i in range(8)]
    ps_rr = [0]

    def next_ps():
        ps_rr[0] = (ps_rr[0] + 1) % 3
        return PS[5 + ps_rr[0]]

    P.dma("sp", ident_f[:], ident_d, w=[ident_f])
    P.op("dve", lambda e: e.tensor_copy(out=ident_b[:], in_=ident_f[:]), r=[ident_f], w=[ident_b])
    P.op("dve", lambda e: e.memset(ones_f[:], 1.0), w=[ones_f])
    P.op("dve", lambda e: e.memset(ones_b[:], 1.0), w=[ones_b])
    for i, v in enumerate((EPS, 256 * EPS, 128 * EPS, 192 * EPS)):
        P.op("dve", (lambda e, i=i, v=v: e.memset(epsc[:, i:i + 1], v)), w=[(epsc, i)])
    epskeys = [(epsc, i) for i in range(4)]

    esA = ExitStack()

    def sbA(name, shape, dt):
        return esA.enter_context(nc.sbuf_tensor(name, list(shape), dt))

    modrep_c = sbA("modrep_c", [128, 2 * D], F32)
    cvec = sbA("cvec", [128, 2, 8], F32)
    svec = sbA("svec", [128, 2, 8], F32)
    srep = sbA("srep", [128, 2, 8, 128], F32)
    brow = sbA("brow", [1, 6 * D], F32)
    g1row = sbA("g1row", [1, D], F32)
    g1rep = sbA("g1rep", [128, D], F32)
    wch = [sbA("wch%d" % i, [128, 8, 512], F32) for i in range(2)]
    tmpA = sbA("tmpA", [128, D], F32)

    P.dma("sp", cvec[:, 0, :], c_d.rearrange("(p k) -> p k", k=8), w=[cvec], semkey="misc_ld", nowaw=True)
    P.dma("sp", cvec[:, 1, :], cctx_d.rearrange("(p k) -> p k", k=8), w=[cvec], semkey="misc_ld", nowaw=True)
    P.dma("sp", brow[:], b_ada_d.rearrange("(o n) -> o n", o=1), w=[brow], semkey="misc_ld", nowaw=True)
    P.dma("sp", g1row[:], g1_d.rearrange("(o n) -> o n", o=1), w=[g1row], semkey="misc_ld", nowaw=True)
    P.op("act", lambda e: e.activation(out=svec[:], in_=cvec[:], func=AF.Silu), r=[cvec], w=[svec])
    P.op("dve", lambda e: e.tensor_copy(out=srep[:], in_=svec[:].unsqueeze(3).to_broadcast([128, 2, 8, 128])),
         r=[svec], w=[srep])
    w_ada_v = w_ada_d.rearrange("(p k) n -> p k n", k=8)
    NCH = 12
    for j in range(NCH):
        wb = wch[j % 2]
        P.dma("sp" if j % 2 == 0 else "act", wb[:], w_ada_v[:, :, j * 512:(j + 1) * 512], w=[wb])
        nvar = 2 if j < 4 else 1
        for v in range(nvar):
            pt = PS[(2 * j + v) % 4]
            for kc in range(8):
                P.op("pe", (lambda e, pt=pt, v=v, kc=kc, wb=wb: e.matmul(pt[:], lhsT=srep[:, v, kc, :], rhs=wb[:, kc, :],
                                                                    start=(kc == 0), stop=False)),
                     r=[srep, wb], w=[pt])
            P.op("pe", (lambda e, pt=pt, j=j: e.matmul(pt[:], lhsT=ones_f[0:1, :], rhs=brow[0:1, j * 512:(j + 1) * 512],
                                                       start=False, stop=True)),
                 r=[ones_f, brow], w=[pt])
            dst = modrep if v == 0 else modrep_c
            P.op("act" if v == 0 else "dve",
                 (lambda e, pt=pt, dst=dst, j=j, v=v: (e.activation(out=dst[:, j * 512:(j + 1) * 512], in_=pt[:], func=AF.Copy)
                                                      if v == 0 else e.tensor_copy(out=dst[:, j * 512:(j + 1) * 512], in_=pt[:]))),
                 r=[pt], w=[(dst, j)])
    for h in range(2):
        pt = PS[4 + h]
        P.op("pe", (lambda e, pt=pt, h=h: e.matmul(pt[:], lhsT=ones_f[0:1, :], rhs=g1row[0:1, h * 512:(h + 1) * 512],
                                                   start=True, stop=True)), r=[ones_f, g1row], w=[pt])
        P.op("dve", (lambda e, pt=pt, h=h: e.tensor_copy(out=g1rep[:, h * 512:(h + 1) * 512], in_=pt[:])),
             r=[pt], w=[(g1rep, h)])

    def diag_extract(dst_ap, src_ap, rkeys, wkey, tmp):
        P.op("dve", lambda e: e.tensor_tensor(out=tmp[:].rearrange("p (c j) -> p c j", j=128),
                                              in0=src_ap.rearrange("p (c j) -> p c j", j=128),
                                              in1=ident_f[:].unsqueeze(1).to_broadcast([128, 8, 128]), op=ALU.mult),
             r=list(rkeys) + [ident_f], w=[tmp])
        P.op("dve", lambda e: e.tensor_reduce(out=dst_ap, in_=tmp[:].rearrange("p (c j) -> p c j", j=128), axis=AX.X, op=ALU.add),
             r=[tmp], w=[wkey])

    Grep = sbA("Grep", [128, D], F32)
    modkeys = [(modrep, j) for j in range(NCH)]
    modckeys = [(modrep_c, j) for j in range(4)]
    g1keys = [(g1rep, 0), (g1rep, 1)]
    P.op("dve", lambda e: e.scalar_tensor_tensor(out=Grep[:], in0=modrep[:, D:2 * D], scalar=1.0, in1=g1rep[:], op0=ALU.add, op1=ALU.mult),
         r=modkeys + g1keys, w=[Grep])
    diag_extract(cols1[:, 0, :], Grep[:], [Grep], (cols1, 0), tmpA)
    diag_extract(cols1[:, 1, :], modrep[:, 0:D], modkeys, (cols1, 1), tmpA)
    P.op("dve", lambda e: e.scalar_tensor_tensor(out=Grep[:], in0=modrep_c[:, D:2 * D], scalar=1.0, in1=g1rep[:], op0=ALU.add, op1=ALU.mult),
         r=modckeys + g1keys, w=[Grep])
    diag_extract(cols1[:, 2, :], Grep[:], [Grep], (cols1, 2), tmpA)
    diag_extract(cols1[:, 3, :], modrep_c[:, 0:D], modckeys, (cols1, 3), tmpA)
    if mod_dbg is not None:
        P.dma("sp", mod_dbg, modrep[:], r=modkeys, w=["mod_dbg"])
    P.flush()
    esA.close()

    esH = ExitStack()
    hT = esH.enter_context(nc.sbuf_tensor("hT", [128, 8, TP], BF16))
    esB = ExitStack()

    def sbB(name, shape, dt):
        return esB.enter_context(nc.sbuf_tensor(name, list(shape), dt))

    xt = [sbB("xt%d" % i, [128, 4, D], F32) for i in range(2)]
    xs = [sbB("xs%d" % i, [128, 4, D], BF16) for i in range(2)]
    junk = sbB("junk", [128, D], BF16)
    ss = [sbB("ss%d" % i, [128, 4], F32) for i in range(2)]
    rstd = [sbB("rstd%d" % i, [128, 4], F32) for i in range(2)]

    zcols = (0, CTX_OFF + CTX, TP - 1)
    for z in zcols:
        P.op("pool", (lambda e, z=z: e.memset(hT[:, :, z:z + 1], 0.0)), w=[(hT, "z%d" % z)])

    groups = [("ctx", 0, 2)] + [("lat", g * 4, 4) for g in range(8)]
    x_v = x_d.rearrange("(n p) d -> p n d", p=128)
    ctx_v = ctx_d.rearrange("(n p) d -> p n d", p=128)

    def load_group(gi):
        seg, t0, n = groups[gi]
        src = (ctx_v if seg == "ctx" else x_v)[:, t0:t0 + n, :]
        P.dma("sp", xt[gi % 2][:, 0:n, :], src, w=[xt[gi % 2]])

    load_group(0)
    for gi, (seg, t0, n) in enumerate(groups):
        if gi + 1 < len(groups):
            load_group(gi + 1)
        X = xt[gi % 2]
        XS = xs[gi % 2]
        SS = ss[gi % 2]
        RS = rstd[gi % 2]
        for i in range(n):
            P.op("act", (lambda e, X=X, SS=SS, i=i: e.activation(out=junk[:], in_=X[:, i, :], func=AF.Square, accum_out=SS[:, i:i + 1])),
                 r=[X], w=[junk, (SS, i)])
        P.op("act", (lambda e, SS=SS, RS=RS, n=n: e.activation(out=RS[:, 0:n], in_=SS[:, 0:n], func=AF.Sqrt, bias=epsc[:, 0:1], scale=1.0 / D)),
             r=[(SS, i) for i in range(n)] + epskeys, w=[RS])
        P.op("dve", (lambda e, RS=RS, n=n: e.reciprocal(out=RS[:, 0:n], in_=RS[:, 0:n])), r=[RS], w=[RS])
        for i in range(n):
            P.op("dve", (lambda e, X=X, XS=XS, RS=RS, i=i: e.tensor_scalar(out=XS[:, i, :], in0=X[:, i, :], scalar1=RS[:, i:i + 1], scalar2=None, op0=ALU.mult)),
                 r=[X, RS], w=[(XS, i)])
        cbase = 0 if seg == "lat" else 2
        off = (LAT_OFF if seg == "lat" else CTX_OFF) + t0 * 128
        for kc in range(8):
            pt = PS[kc % 4]
            ptb = pt[:].bitcast(BF16)
            for i in range(n):
                P.op("pe", (lambda e, ptb=ptb, XS=XS, i=i, kc=kc: e.transpose(out=ptb[:, i * 128:(i + 1) * 128], in_=XS[:, i, kc * 128:(kc + 1) * 128], identity=ident_b[:])),
                     r=[(XS, i), ident_b], w=[pt])
            if kc % 2 == 0:
                P.op("act", (lambda e, ptb=ptb, kc=kc, off=off, n=n, cbase=cbase: e.activation(
                    out=hT[:, kc, off:off + n * 128], in_=ptb[:, 0:n * 128], func=AF.Identity,
                    scale=cols1[:, cbase, kc:kc + 1], bias=cols1[:, cbase + 1, kc:kc + 1])),
                    r=[pt, (cols1, cbase), (cols1, cbase + 1)], w=[(hT, gi, kc)])
            else:
                P.op("dve", (lambda e, ptb=ptb, kc=kc, off=off, n=n, cbase=cbase: e.tensor_scalar(
                    out=hT[:, kc, off:off + n * 128], in0=ptb[:, 0:n * 128],
                    scalar1=cols1[:, cbase, kc:kc + 1], scalar2=cols1[:, cbase + 1, kc:kc + 1], op0=ALU.mult, op1=ALU.add)),
                    r=[pt, (cols1, cbase), (cols1, cbase + 1)], w=[(hT, gi, kc)])
    hkeys = [(hT, gi, kc) for gi in range(len(groups)) for kc in range(8)] + [(hT, "z%d" % z) for z in zcols]
    if hT_dbg is not None:
        P.dma("sp", hT_dbg, hT[:], r=hkeys, w=["hT_dbg"])
    P.flush()
    esB.close()
    if stop_after == "B":
        P.op("sp", lambda e: e.nop(), r=["hT_dbg", "mod_dbg"])
        P.flush(final_wait_keys=["hT_dbg", "mod_dbg"])
        esH.close()
        es.close()
        return nc

    esC = ExitStack()

    def sbC(name, shape, dt):
        return esC.enter_context(nc.sbuf_tensor(name, list(shape), dt))

    w_in_b = sbC("w_in_b", [128, 8, IN_COLS], BF16)
    wtap = sbC("wtap", [128, 3, 8, 512], BF16)
    wkr_sw = sbC("wkr_sw", [128, 8, 64], BF16)
    w_uq_b = sbC("w_uq_b", [128, 2, 768], BF16)
    w_uq_sw = sbC("w_uq_sw", [128, 2, 4, 64], BF16)
    w_ukv_b = sbC("w_ukv_b", [128, 1024], BF16)
    cosT = sbC("cosT_s", [64, NT], BF16)
    sinT = sbC("sinT_s", [64, NT], BF16)
    convrep = sbC("convrep", [128, 3, 512], F32)
    gcol = sbC("gcol", [128, 16], F32)
    brow16 = sbC("brow16", [1, 16], F32)

    w_in_v = w_in_d.rearrange("(k p) n -> p k n", p=128)
    for kc in range(8):
        P.dma("pool", w_in_b[:, kc, :], w_in_v[:, kc, :], w=[(w_in_b, kc)], semkey="w_in_ld", nowaw=True)
    winkeys = [(w_in_b, kc) for kc in range(8)]
    P.dma("pool", w_uq_b[:], w_uq_d.rearrange("(k p) n -> p k n", p=128), w=[w_uq_b], semkey="misc_ld", nowaw=True)
    P.dma("pool", w_ukv_b[:], w_ukv_d, w=[w_ukv_b], semkey="misc_ld", nowaw=True)
    P.dma("pool", cosT[:], cosT_d, w=[cosT], semkey="misc_ld", nowaw=True)
    P.dma("pool", sinT[:], sinT_d, w=[sinT], semkey="misc_ld", nowaw=True)
    def colsrc(d, a0, n_):
        return d[a0:a0 + n_].rearrange("(p o) -> p o", o=1)
    gsrc = [(0, g_cq_d, 0, 128, 0), (1, g_cq_d, 128, 128, 0), (2, g_ckv_d, 0, 128, 0),
            (3, g_qn_d, 0, 128, 0), (4, g_qn_d, 128, 64, 0), (6, g_kn_d, 0, 128, 0), (7, g_kn_d, 128, 64, 0)]
    for (d0, s0) in [(0, 16), (16, 0), (32, 48), (48, 32)]:
        gsrc.append((5, g_qn_d, 128 + s0, 16, d0))
        gsrc.append((8, g_kn_d, 128 + s0, 16, d0))
    P.op("dve", lambda e: e.memset(gcol[:], 0.0), w=[gcol])
    for gi_, (ci, d, a0, n_, p0) in enumerate(gsrc):
        P.dma("sp", gcol[p0:p0 + n_, ci:ci + 1], colsrc(d, a0, n_), r=[gcol], w=[(gcol, "ld", gi_)], semkey="gcol_ld", nowaw=True)
    gldkeys = [(gcol, "ld", gi_) for gi_ in range(len(gsrc))]
    GMUL = {0: 16.0, 1: 16.0, 2: math.sqrt(128.0), 6: math.sqrt(192.0), 7: math.sqrt(192.0), 8: math.sqrt(192.0)}
    for ci, mul in GMUL.items():
        P.op("dve", (lambda e, ci=ci, mul=mul: e.tensor_scalar(out=gcol[:, ci:ci + 1], in0=gcol[:, ci:ci + 1], scalar1=mul, scalar2=None, op0=ALU.mult)),
             r=gldkeys, w=[(gcol, ci)])
    gkeys = gldkeys + [(gcol, ci) for ci in GMUL]
    P.dma("sp", brow16[0:1, 0:8], b_ig_d.rearrange("(o n) -> o n", o=1), w=[(brow16, 0)], semkey="misc_ld", nowaw=True)
    P.dma("sp", brow16[0:1, 8:16], b_fg_d.rearrange("(o n) -> o n", o=1), w=[(brow16, 1)], semkey="misc_ld", nowaw=True)
    P.dma("sp", convrep[:], conv_d.rearrange("(o j) n -> o j n", o=1).to_broadcast([128, 3, 512]), w=[convrep], semkey="misc_ld", nowaw=True)
    for j in range(3):
        P.op("dve", (lambda e, j=j: e.tensor_tensor(out=wtap[:, j, :, :], in0=w_in_b[:, :, ML_Q0:ML_Q0 + 512],
                                                    in1=convrep[:, j, :].unsqueeze(1).to_broadcast([128, 8, 512]), op=ALU.mult)),
             r=winkeys + [convrep], w=[(wtap, j)])
    tapkeys = [(wtap, j) for j in range(3)]
    sw_blocks = [(0, 16), (16, 0), (32, 48), (48, 32)]
    for (d0, s0) in sw_blocks:
        P.op("pool", (lambda e, d0=d0, s0=s0: e.tensor_copy(out=wkr_sw[:, :, d0:d0 + 16], in_=w_in_b[:, :, C_KR0 + s0:C_KR0 + s0 + 16])),
             r=winkeys, w=[(wkr_sw, d0)])
        P.op("pool", (lambda e, d0=d0, s0=s0: e.tensor_copy(
            out=w_uq_sw[:, :, :, d0:d0 + 16],
            in_=w_uq_b[:].rearrange("p k (h c) -> p k h c", c=192)[:, :, :, 128 + s0:128 + s0 + 16])),
            r=[w_uq_b], w=[(w_uq_sw, d0)])
    krswkeys = [(wkr_sw, d0) for d0, _ in sw_blocks]
    uqswkeys = [(w_uq_sw, d0) for d0, _ in sw_blocks]

    sqA = [sbC("sqA%d" % i, [128, 512], BF16) for i in range(3)]
    rrep = [sbC("rrep%d" % i, [128, 512], F32) for i in range(2)]
    cqn = sbC("cqn", [128, 2, 512], BF16)
    ckvn = sbC("ckvn", [128, 512], BF16)
    krsq = sbC("krsq", [64, 512], BF16)
    onT = [sbC("onT%d" % i, [128, 512], BF16) for i in range(2)]
    orT = [sbC("orT%d" % i, [64, 512], BF16) for i in range(2)]
    t1 = sbC("t1", [64, 512], F32)
    t2 = sbC("t2", [64, 512], F32)
    fmT = [sbC("fmT%d" % i, [128, 512], BF16) for i in range(2)]
    tokb = [sbC("tokb%d" % i, [128, 512], BF16) for i in range(3)]
    gat = [sbC("gat%d" % i, [128, 16], F32) for i in range(2)]
    cnt = {"sq": 0, "rr": 0, "on": 0, "or": 0, "fm": 0, "tok": 0, "gat": 0}

    def rot(lst, key):
        cnt[key] += 1
        return lst[cnt[key] % len(lst)]

    blocks = [("ctx", 0, 256)] + [("lat", b * 512, 512) for b in range(8)]

    def hcols(seg, t0, n, kc, shift=0):
        off = (LAT_OFF if seg == "lat" else CTX_OFF) + t0 + shift
        return hT[:, kc, off:off + n]

    def proj8(pt_ap, wfn, seg, t0, n, rkeys, wkey, taps=False):
        k = 0
        tot = 24 if taps else 8
        for j in (range(3) if taps else [1]):
            for kc in range(8):
                P.op("pe", (lambda e, j=j, kc=kc, k=k: e.matmul(pt_ap, lhsT=wfn(j, kc), rhs=hcols(seg, t0, n, kc, j - 1),
                                                             start=(k == 0), stop=(k == tot - 1))),
                     r=list(rkeys) + hkeys, w=[wkey])
                k += 1

    def rsqrt_rep(pt, n, eps_col, dst):
        P.op("act", (lambda e: e.activation(out=dst[:, 0:n], in_=pt[:, 0:n], func=AF.Sqrt, bias=epsc[:, eps_col:eps_col + 1], scale=1.0)),
             r=[pt] + epskeys, w=[dst])
        P.op("dve", (lambda e: e.reciprocal(out=dst[:, 0:n], in_=dst[:, 0:n])), r=[dst], w=[dst])

    def head_prep(ptn, ptr, ptrs, sq_r_tile, gn, gr, grs, n, tg0, dstn_d, dstr_d):
        sqn = rot(sqA, "sq")
        P.op("act", (lambda e: e.activation(out=sqn[:, 0:n], in_=ptn[:, 0:n], func=AF.Square)), r=[ptn], w=[sqn])
        pss = next_ps()
        P.op("pe", (lambda e: e.matmul(pss[:, 0:n], lhsT=ones_b[:, :], rhs=sqn[:, 0:n], start=True, stop=False)), r=[ones_b, sqn], w=[pss])
        P.op("pe", (lambda e: e.matmul(pss[:, 0:n], lhsT=ones_b[0:64, :], rhs=sq_r_tile[0:64, 0:n], start=False, stop=True)),
             r=[ones_b, sq_r_tile], w=[pss])
        rr = rot(rrep, "rr")
        rsqrt_rep(pss, n, 3, rr)
        on = rot(onT, "on")
        P.op("dve", (lambda e: e.scalar_tensor_tensor(out=on[:, 0:n], in0=ptn[:, 0:n], scalar=gcol[:, gn:gn + 1], in1=rr[:, 0:n], op0=ALU.mult, op1=ALU.mult)),
             r=[ptn, rr] + gkeys, w=[on])
        P.dma("sp", dstn_d, on[:, 0:n], r=[on], w=["mla_scr"], nowaw=True)
        orr = rot(orT, "or")
        P.op("dve", (lambda e: e.scalar_tensor_tensor(out=t1[:, 0:n], in0=ptr[0:64, 0:n], scalar=gcol[0:64, gr:gr + 1], in1=cosT[:, tg0:tg0 + n], op0=ALU.mult, op1=ALU.mult)),
             r=[ptr, cosT] + gkeys, w=[t1])
        P.op("dve", (lambda e: e.scalar_tensor_tensor(out=t2[:, 0:n], in0=ptrs[0:64, 0:n], scalar=gcol[0:64, grs:grs + 1], in1=sinT[:, tg0:tg0 + n], op0=ALU.mult, op1=ALU.mult)),
             r=[ptrs, sinT] + gkeys, w=[t2])
        P.op("pool", (lambda e: e.tensor_tensor(out=t1[:, 0:n], in0=t1[:, 0:n], in1=t2[:, 0:n], op=ALU.add)), r=[t1, t2], w=[t1])
        P.op("dve", (lambda e: e.tensor_tensor(out=orr[:, 0:n], in0=t1[:, 0:n], in1=rr[0:64, 0:n], op=ALU.mult)), r=[t1, rr], w=[orr])
        P.dma("sp", dstr_d, orr[:, 0:n], r=[orr], w=["mla_scr"], nowaw=True)

    def block_body(bi, seg, t0, n):
        tg0 = t0 if seg == "ctx" else CTX + t0
        ntile = n // 128
        pq = [PS[0], PS[1]]
        if seg == "lat":
            for cc in range(2):
                proj8(pq[cc][:, 0:n], (lambda j, kc, cc=cc: w_in_b[:, kc, C_Q0 + cc * 128:C_Q0 + (cc + 1) * 128]), seg, t0, n, winkeys, pq[cc])
        pkv = PS[2]
        proj8(pkv[:, 0:n], (lambda j, kc: w_in_b[:, kc, C_KV0:C_KV0 + 128]), seg, t0, n, winkeys, pkv)
        pkr = PS[3]
        proj8(pkr[0:64, 0:n], (lambda j, kc: w_in_b[:, kc, C_KR0:C_KR0 + 64]), seg, t0, n, winkeys, pkr)
        pkrs = PS[4]
        proj8(pkrs[0:64, 0:n], (lambda j, kc: wkr_sw[:, kc, :]), seg, t0, n, krswkeys, pkrs)
        if seg == "lat":
            sq0, sq1 = rot(sqA, "sq"), rot(sqA, "sq")
            for cc, sq in ((0, sq0), (1, sq1)):
                P.op("act", (lambda e, cc=cc, sq=sq: e.activation(out=sq[:, 0:n], in_=pq[cc][:, 0:n], func=AF.Square)), r=[pq[cc]], w=[sq])
            pss = next_ps()
            P.op("pe", (lambda e, pss=pss, sq0=sq0: e.matmul(pss[:, 0:n], lhsT=ones_b[:, :], rhs=sq0[:, 0:n], start=True, stop=False)), r=[ones_b, sq0], w=[pss])
            P.op("pe", (lambda e, pss=pss, sq1=sq1: e.matmul(pss[:, 0:n], lhsT=ones_b[:, :], rhs=sq1[:, 0:n], start=False, stop=True)), r=[ones_b, sq1], w=[pss])
            rr = rot(rrep, "rr")
            rsqrt_rep(pss, n, 1, rr)
            for cc in range(2):
                P.op("dve", (lambda e, cc=cc, rr=rr: e.scalar_tensor_tensor(out=cqn[:, cc, 0:n], in0=pq[cc][:, 0:n], scalar=gcol[:, cc:cc + 1], in1=rr[:, 0:n], op0=ALU.mult, op1=ALU.mult)),
                     r=[pq[cc], rr] + gkeys, w=[(cqn, cc)])
        sqk = rot(sqA, "sq")
        P.op("act", (lambda e, sqk=sqk: e.activation(out=sqk[:, 0:n], in_=pkv[:, 0:n], func=AF.Square)), r=[pkv], w=[sqk])
        pss = next_ps()
        P.op("pe", (lambda e, pss=pss, sqk=sqk: e.matmul(pss[:, 0:n], lhsT=ones_b[:, :], rhs=sqk[:, 0:n], start=True, stop=True)), r=[ones_b, sqk], w=[pss])
        rr = rot(rrep, "rr")
        rsqrt_rep(pss, n, 2, rr)
        P.op("dve", (lambda e, rr=rr: e.scalar_tensor_tensor(out=ckvn[:, 0:n], in0=pkv[:, 0:n], scalar=gcol[:, 2:3], in1=rr[:, 0:n], op0=ALU.mult, op1=ALU.mult)),
             r=[pkv, rr] + gkeys, w=[ckvn])
        P.op("act", (lambda e: e.activation(out=krsq[:, 0:n], in_=pkr[0:64, 0:n], func=AF.Square)), r=[pkr], w=[krsq])
        for h in range(4):
            pkn = next_ps()
            P.op("pe", (lambda e, pkn=pkn, h=h: e.matmul(pkn[:, 0:n], lhsT=w_ukv_b[:, h * 256:h * 256 + 128], rhs=ckvn[:, 0:n], start=True, stop=True)),
                 r=[w_ukv_b, ckvn], w=[pkn])
            head_prep(pkn, pkr, pkrs, krsq, 6, 7, 8, n, tg0, KnT_d[h, :, tg0:tg0 + n], KrT_d[h, :, tg0:tg0 + n])
        for i in range(ntile):
            pv = next_ps()
            P.op("pe", (lambda e, pv=pv, i=i: e.matmul(pv[:].rearrange("p (h c) -> p h c", c=128), lhsT=ckvn[:, i * 128:(i + 1) * 128],
                                                       rhs=w_ukv_b[:].rearrange("p (h c) -> p h c", c=256)[:, :, 128:256], start=True, stop=True)),
                 r=[w_ukv_b, ckvn], w=[pv])
            tb = rot(tokb, "tok")
            P.op("act", (lambda e, pv=pv, tb=tb: e.activation(out=tb[:], in_=pv[:], func=AF.Copy)), r=[pv], w=[tb])
            P.dma("sp", Vt_d[tg0 + i * 128:tg0 + (i + 1) * 128, :], tb[:], r=[tb], w=["mla_scr"], nowaw=True)
        if seg == "lat":
            for h in range(4):
                pqn = PS[0]
                for cc in range(2):
                    P.op("pe", (lambda e, pqn=pqn, h=h, cc=cc: e.matmul(pqn[:, 0:n], lhsT=w_uq_b[:, cc, h * 192:h * 192 + 128], rhs=cqn[:, cc, 0:n], start=(cc == 0), stop=(cc == 1))),
                         r=[w_uq_b, (cqn, 0), (cqn, 1)], w=[pqn])
                pqr = PS[1]
                for cc in range(2):
                    P.op("pe", (lambda e, pqr=pqr, h=h, cc=cc: e.matmul(pqr[0:64, 0:n], lhsT=w_uq_b[:, cc, h * 192 + 128:h * 192 + 192], rhs=cqn[:, cc, 0:n], start=(cc == 0), stop=(cc == 1))),
                         r=[w_uq_b, (cqn, 0), (cqn, 1)], w=[pqr])
                pqrs = PS[2]
                for cc in range(2):
                    P.op("pe", (lambda e, pqrs=pqrs, h=h, cc=cc: e.matmul(pqrs[0:64, 0:n], lhsT=w_uq_sw[:, cc, h, :], rhs=cqn[:, cc, 0:n], start=(cc == 0), stop=(cc == 1))),
                         r=uqswkeys + [(cqn, 0), (cqn, 1)], w=[pqrs])
                qsq = rot(sqA, "sq")
                P.op("act", (lambda e, qsq=qsq, pqr=pqr: e.activation(out=qsq[0:64, 0:n], in_=pqr[0:64, 0:n], func=AF.Square)), r=[pqr], w=[qsq])
                head_prep(pqn, pqr, pqrs, qsq, 3, 4, 5, n, tg0, QnT_d[h, :, t0:t0 + n], QrT_d[h, :, t0:t0 + n])
        for cc in range(4):
            pf = next_ps()
            proj8(pf[:, 0:n], (lambda j, kc, cc=cc: wtap[:, j, kc, cc * 128:(cc + 1) * 128]), seg, t0, n, tapkeys, pf, taps=True)
            fm = rot(fmT, "fm")
            P.op("act", (lambda e, pf=pf, fm=fm: e.activation(out=fm[:, 0:n], in_=pf[:, 0:n], func=AF.Silu)), r=[pf], w=[fm])
            if cc < 2:
                P.op("pool", (lambda e, fm=fm: e.tensor_scalar(out=fm[:, 0:n], in0=fm[:, 0:n], scalar1=0.125, scalar2=None, op0=ALU.mult)), r=[fm], w=[fm])
                for hh in range(2):
                    P.dma("sp", mqT_d[2 * cc + hh, :, tg0:tg0 + n], fm[hh * 64:(hh + 1) * 64, 0:n], r=[fm], w=["ml_scr"], nowaw=True)
            else:
                for hh in range(2):
                    P.dma("sp", mkT_d[2 * (cc - 2) + hh, :, tg0:tg0 + n], fm[hh * 64:(hh + 1) * 64, 0:n], r=[fm], w=["ml_scr"], nowaw=True)
        for i in range(ntile):
            tt = t0 + i * 128
            tg = tg0 + i * 128
            pk = next_ps()
            k = 0
            for j in range(3):
                for kc in range(8):
                    P.op("pe", (lambda e, pk=pk, j=j, kc=kc, k=k, tt=tt: e.matmul(pk[:, 0:256], lhsT=hcols(seg, tt, 128, kc, j - 1), rhs=wtap[:, j, kc, 256:512],
                                                                           start=(k == 0), stop=(k == 23))),
                         r=tapkeys + hkeys, w=[pk])
                    k += 1
            tb = rot(tokb, "tok")
            P.op("act", (lambda e, pk=pk, tb=tb: e.activation(out=tb[:, 0:256], in_=pk[:, 0:256], func=AF.Silu)), r=[pk], w=[tb])
            P.dma("sp", mk_d[tg:tg + 128, :], tb[:, 0:256], r=[tb], w=["ml_scr"], nowaw=True)
            pv = next_ps()
            for kc in range(8):
                P.op("pe", (lambda e, pv=pv, kc=kc, tt=tt: e.matmul(pv[:], lhsT=hcols(seg, tt, 128, kc), rhs=w_in_b[:, kc, ML_V0:ML_V0 + 512], start=(kc == 0), stop=(kc == 7))),
                     r=winkeys + hkeys, w=[pv])
            tb = rot(tokb, "tok")
            P.op("dve", (lambda e, pv=pv, tb=tb: e.tensor_copy(out=tb[:], in_=pv[:])), r=[pv], w=[tb])
            P.dma("sp", mv_d[tg:tg + 128, :], tb[:], r=[tb], w=["ml_scr"], nowaw=True)
            if seg == "lat":
                po = next_ps()
                for kc in range(8):
                    P.op("pe", (lambda e, po=po, kc=kc, tt=tt: e.matmul(po[:], lhsT=hcols(seg, tt, 128, kc), rhs=w_in_b[:, kc, ML_O0:ML_O0 + 512], start=(kc == 0), stop=(kc == 7))),
                         r=winkeys + hkeys, w=[po])
                tb = rot(tokb, "tok")
                P.op("act", (lambda e, po=po, tb=tb: e.activation(out=tb[:], in_=po[:], func=AF.Sigmoid)), r=[po], w=[tb])
                P.dma("sp", mo_d[tt:tt + 128, :], tb[:], r=[tb], w=["ml_scr"], nowaw=True)
            pg = next_ps()
            for kc in range(8):
                P.op("pe", (lambda e, pg=pg, kc=kc, tt=tt: e.matmul(pg[:, 0:16], lhsT=hcols(seg, tt, 128, kc), rhs=w_in_b[:, kc, ML_G0:ML_G0 + 16], start=(kc == 0), stop=False)),
                     r=winkeys + hkeys, w=[pg])
            P.op("pe", (lambda e, pg=pg: e.matmul(pg[:, 0:16], lhsT=ones_f[0:1, :], rhs=brow16[0:1, :], start=False, stop=True)), r=[ones_f, (brow16, 0), (brow16, 1)], w=[pg])
            gt = rot(gat, "gat")
            P.op("dve", (lambda e, pg=pg, gt=gt: e.tensor_copy(out=gt[:], in_=pg[:, 0:16])), r=[pg], w=[gt])
            P.dma("sp", mg_d[tg:tg + 128, :], gt[:], r=[gt], w=["ml_scr"], nowaw=True)

    for bi, (seg, t0, n) in enumerate(blocks):
        block_body(bi, seg, t0, n)
    P.flush()
    esC.close()
    esH.close()
    if stop_after == "C":
        P.op("sp", lambda e: e.nop(), r=["mla_scr", "ml_scr"])
        P.flush(final_wait_keys=["mla_scr", "ml_scr"])
        es.close()
        return nc

    esD = ExitStack()

    def sbD(name, shape, dt):
        return esD.enter_context(nc.sbuf_tensor(name, list(shape), dt))

    maskf = sbD("maskf", [128, 128], F32)
    maskb = sbD("maskb", [128, 128], F32)
    Gt = sbD("Gt", [128, NTILE, 16], F32)
    nlf = sbD("nlf", [128, NTILE, 8], F32)
    ncf = sbD("ncf", [128, NTILE, 8], F32)
    nFt = sbD("nFt", [128, NTILE, 8], F32)
    colw = sbD("colw", [128, NTILE, 8], F32)
    flo = sbD("flo", [128, NTILE, 8], F32)
    dec = sbD("dec", [128, NTILE, 8], F32)
    gmlrep = sbD("gmlrep", [128, 512], F32)
    hbuf = sbD("hbuf", [128, 32, 4, 128], F32)
    Cst = [sbD("Cst%d" % d, [64, 4, 132], F32) for d in range(2)]
    P.dma("sp", maskf[:], maskf_d, w=[maskf], semkey="misc_ld", nowaw=True)
    P.dma("sp", maskb[:], maskb_d, w=[maskb], semkey="misc_ld", nowaw=True)
    P.dma("sp", gmlrep[:], g_ml_d.rearrange("(o n) -> o n", o=1).to_broadcast([128, 512]), w=[gmlrep], semkey="misc_ld", nowaw=True)
    P.dma("sp", Gt[:], mg_d.rearrange("(n p) c -> p n c", p=128), r=["ml_scr"], w=[Gt])
    for d in range(2):
        P.op("pool", (lambda e, d=d: e.memset(Cst[d][:], 0.0)), w=[(Cst[d], h) for h in range(4)])
    P.op("act", lambda e: e.activation(out=nlf[:], in_=Gt[:, :, 8:16], func=AF.Exp, scale=-1.0), r=[Gt], w=[nlf])
    P.op("act", lambda e: e.activation(out=nlf[:], in_=nlf[:], func=AF.Ln, bias=ones_f[:, 0:1], scale=1.0), r=[nlf, ones_f], w=[nlf])
    pcf = PS[0]
    nlf_s = sbD("nlf_s", [128, 2, NTILE, 4], F32)
    for d_ in range(2):
        P.op("dve", (lambda e, d_=d_: e.tensor_copy(out=nlf_s[:, d_, :, :], in_=nlf[:, :, d_ * 4:d_ * 4 + 4])), r=[nlf], w=[(nlf_s, d_)])
    P.op("pe", lambda e: e.matmul(pcf[:, 0:136], lhsT=maskf[:], rhs=nlf_s[:, 0, :, :].rearrange("p n h -> p (n h)"), start=True, stop=True), r=[maskf, (nlf_s, 0)], w=[pcf])
    P.op("pe", lambda e: e.matmul(pcf[:, 136:272], lhsT=maskb[:], rhs=nlf_s[:, 1, :, :].rearrange("p n h -> p (n h)"), start=True, stop=True), r=[maskb, (nlf_s, 1)], w=[pcf])
    P.op("dve", lambda e: e.tensor_copy(out=ncf[:, :, 0:4], in_=pcf[:, 0:136].rearrange("p (n h) -> p n h", h=4)), r=[pcf], w=[(ncf, 0)])
    P.op("dve", lambda e: e.tensor_copy(out=ncf[:, :, 4:8], in_=pcf[:, 136:272].rearrange("p (n h) -> p n h", h=4)), r=[pcf], w=[(ncf, 1)])
    pft = PS[1]
    P.op("pe", lambda e: e.matmul(pft[:, 0:272], lhsT=ones_f[:], rhs=nlf[:].rearrange("p n h -> p (n h)"), start=True, stop=True), r=[ones_f, nlf], w=[pft])
    P.op("dve", lambda e: e.tensor_copy(out=nFt[:].rearrange("p n h -> p (n h)"), in_=pft[:, 0:272]), r=[pft], w=[nFt])
    P.op("dve", lambda e: e.tensor_tensor(out=flo[:], in0=ncf[:], in1=nFt[:], op=ALU.subtract), r=[(ncf, 0), (ncf, 1), nFt], w=[flo])
    P.op("dve", lambda e: e.tensor_tensor(out=colw[:], in0=flo[:], in1=Gt[:, :, 0:8], op=ALU.add), r=[flo, Gt], w=[colw])
    P.op("act", lambda e: e.activation(out=flo[:], in_=flo[:], func=AF.Exp), r=[flo], w=[flo])
    P.op("act", lambda e: e.activation(out=colw[:], in_=colw[:], func=AF.Exp), r=[colw], w=[colw])
    P.op("act", lambda e: e.activation(out=dec[:], in_=nFt[:], func=AF.Exp, scale=-1.0), r=[nFt], w=[dec])

    if "colw_dbg" in debug:
        colw_dbg = dscratch("colw_dbg", [3, 128, NTILE, 8], F32)
        P.dma("sp", colw_dbg[0], colw[:], r=[colw], w=["colw_dbg"], nowaw=True)
        P.dma("sp", colw_dbg[1], flo[:], r=[flo], w=["colw_dbg"], nowaw=True)
        P.dma("sp", colw_dbg[2], dec[:], r=[dec], w=["colw_dbg"], nowaw=True)
    if stop_after == "D0":
        P.op("sp", lambda e: e.nop(), r=["colw_dbg"])
        P.flush(final_wait_keys=["colw_dbg"])
        esD.close()
        es.close()
        return nc
    NB = 2
    qTt = [[sbD("qTt%d_%d" % (d, i), [64, 4, 128], BF16) for i in range(NB)] for d in range(2)]
    kTt = [[sbD("kTt%d_%d" % (d, i), [64, 4, 128], BF16) for i in range(NB)] for d in range(2)]
    ktk = [[sbD("ktk%d_%d" % (d, i), [128, 256], BF16) for i in range(NB)] for d in range(2)]
    vau = [[sbD("vau%d_%d" % (d, i), [128, 4, 132], BF16) for i in range(NB)] for d in range(2)]
    vw = [[sbD("vw%d_%d" % (d, i), [128, 4, 132], BF16) for i in range(NB)] for d in range(2)]
    pT = [[sbD("pT%d_%d" % (d, i), [128, 4, 128], BF16) for i in range(NB)] for d in range(2)]
    Bbf = [[sbD("Bbf%d_%d" % (d, i), [64, 4, 132], BF16) for i in range(NB)] for d in range(2)]
    sgo = [sbD("sgo%d" % i, [128, 512], BF16) for i in range(2)]
    hs = [sbD("hs%d" % i, [128, 4, 128], F32) for i in range(2)]
    hjunk = sbD("hjunk", [128, 128], BF16)
    dd = [sbD("dd%d" % i, [128, 4], F32) for i in range(4)]
    ssn = [sbD("ssn%d" % i, [128, 4], F32) for i in range(2)]
    mixb = [sbD("mixb%d" % i, [128, 512], BF16) for i in range(2)]
    mixTs = [sbD("mixTs%d" % i, [128, 4, 128], BF16) for i in range(2)]
    for d in range(2):
        for i in range(NB):
            P.op("pool", (lambda e, d=d, i=i: e.memset(vau[d][i][:, :, 128:132], 0.0)), w=[(vau[d][i], "one")])
            P.op("pool", (lambda e, d=d, i=i: e.memset(vau[d][i][:, :, 128:129], 1.0)), r=[(vau[d][i], "one")], w=[(vau[d][i], "one")])
    ddc = [0]
    fin = [0]

    def order(d, j):
        if d == 0:
            return j
        return 1 - j if j < 2 else 35 - j

    def ml_load(d, j):
        g = order(d, j)
        i = j % NB
        tsl = slice(g * 128, (g + 1) * 128)
        sk = "mlld%d_%d" % (d, i)
        allw = [qTt[d][i], kTt[d][i], ktk[d][i], (vau[d][i], "v")]
        P.dma("sp", qTt[d][i][:], mqT_d[:, :, tsl].rearrange("h p t -> p h t"), r=["ml_scr"], w=allw, semkey=sk, nowaw=True)
        P.dma("sp", kTt[d][i][:], mkT_d[:, :, tsl].rearrange("h p t -> p h t"), r=["ml_scr"], w=allw, semkey=sk, nowaw=True)
        P.dma("act", ktk[d][i][:], mk_d[tsl, :], r=["ml_scr"], w=allw, semkey=sk, nowaw=True)
        P.dma("act", vau[d][i][:, :, 0:128], mv_d[tsl, :].rearrange("t (h c) -> t h c", c=128), r=["ml_scr"], w=allw, semkey=sk, nowaw=True)

    def ml_step(d, j):
        g = order(d, j)
        i = j % NB
        lat = g >= 2
        gl = g - 2
        QT, KT, KK, VA, VW, PT, BB = qTt[d][i], kTt[d][i], ktk[d][i], vau[d][i], vw[d][i], pT[d][i], Bbf[d][i]
        vakeys = [(VA, "one"), (VA, "v")]
        mask = maskf if d == 0 else maskb
        C = Cst[d]
        for h in range(4):
            P.op("dve", (lambda e, h=h: e.tensor_scalar(out=VW[:, h, :], in0=VA[:, h, :], scalar1=colw[:, g, d * 4 + h:d * 4 + h + 1], scalar2=None, op0=ALU.mult)),
                 r=vakeys + [colw], w=[(VW, h)])
        if lat:
            pS = PS[d]
            for h in range(4):
                c, po = h // 2, (h % 2) * 64
                P.op("pe", (lambda e, h=h, c=c, po=po: e.matmul(pS[:, h * 128:(h + 1) * 128], lhsT=KT[:, h, :], rhs=QT[:, h, :], start=True, stop=True)),
                     r=[KT, QT], w=[pS])
            for h in range(4):
                P.op("dve", (lambda e, h=h: e.scalar_tensor_tensor(out=PT[:, h, :], in0=pS[:, h * 128:(h + 1) * 128], scalar=colw[:, g, d * 4 + h:d * 4 + h + 1], in1=mask[:], op0=ALU.mult, op1=ALU.mult)),
                     r=[pS, colw, mask], w=[(PT, h)])
            for h in range(4):
                po = (h % 2) * 64
                P.op("act", (lambda e, h=h, po=po: e.activation(out=BB[:, h, :], in_=C[:, h, :], func=AF.Copy, scale=dec[0:64, g, d * 4 + h:d * 4 + h + 1])),
                     r=[(C, h), dec], w=[(BB, h)])
            pO = [PS[4], PS[5]]
            for h in range(4):
                c, po = h // 2, (h % 2) * 64
                ob = pO[h // 2][:, (h % 2) * 256:(h % 2) * 256 + 132]
                P.op("pe", (lambda e, h=h, ob=ob: e.matmul(ob, lhsT=PT[:, h, :], rhs=VA[:, h, :], start=True, stop=False)),
                     r=[(PT, h)] + vakeys, w=[pO[h // 2]])
                P.op("pe", (lambda e, h=h, ob=ob, c=c, po=po: e.matmul(ob, lhsT=QT[:, h, :], rhs=BB[:, h, :], start=False, stop=True)),
                     r=[QT, (BB, h)], w=[pO[h // 2]])
        pD = [PS[2], PS[3]]
        for h in range(4):
            c = h // 2
            ob = pD[c][0:64, (h % 2) * 256:(h % 2) * 256 + 132]
            P.op("pe", (lambda e, h=h, c=c, ob=ob: e.matmul(ob, lhsT=KK[:, h * 64:(h + 1) * 64], rhs=VW[:, h, :], start=True, stop=True)),
                 r=[KK, (VW, h)], w=[pD[c]])
        for h in range(4):
            c, po = h // 2, (h % 2) * 64
            P.op("dve", (lambda e, h=h, c=c, po=po: e.scalar_tensor_tensor(out=C[:, h, :], in0=C[:, h, :], scalar=dec[0:64, g, d * 4 + h:d * 4 + h + 1],
                                                                       in1=pD[c][0:64, (h % 2) * 256:(h % 2) * 256 + 132], op0=ALU.mult, op1=ALU.add)),
                 r=[(C, h), dec, pD[c]], w=[(C, h)])
        if not lat:
            return
        ddc[0] += 1
        DD = dd[ddc[0] % 4]
        for c in range(2):
            P.op("act", (lambda e, c=c: e.activation(out=DD[:, 2 * c:2 * c + 2], in_=pO[c][:, 0:512].rearrange("p (h n) -> p h n", n=256)[:, :, 128], func=AF.Abs)),
                 r=[pO[c]], w=[(DD, c)])
            P.op("dve", (lambda e, c=c: e.tensor_tensor(out=DD[:, 2 * c:2 * c + 2], in0=DD[:, 2 * c:2 * c + 2],
                                                        in1=flo[:, g, d * 4 + 2 * c:d * 4 + 2 * c + 2], op=ALU.max)),
                 r=[(DD, c), flo], w=[(DD, c)])
        P.op("dve", (lambda e: e.reciprocal(out=DD[:], in_=DD[:])), r=[(DD, 0), (DD, 1)], w=[DD])
        first = (g <= 17) == (d == 0)
        if first:
            for h in range(4):
                P.op("dve", (lambda e, h=h: e.tensor_scalar(out=hbuf[:, gl, h, :], in0=pO[h // 2][:, (h % 2) * 256:(h % 2) * 256 + 128], scalar1=DD[:, h:h + 1], scalar2=None, op0=ALU.mult)),
                     r=[pO[h // 2], DD], w=[(hbuf, gl, h)])
            return
        fin[0] += 1
        fi = fin[0] % 2
        HS, SSN, MB, MT, SG = hs[fi], ssn[fi], mixb[fi], mixTs[fi], sgo[fi]
        P.dma("act", SG[:], mo_d[gl * 128:(gl + 1) * 128, :], r=["ml_scr"], w=[SG])
        for h in range(4):
            P.op("dve", (lambda e, h=h: e.scalar_tensor_tensor(out=HS[:, h, :], in0=pO[h // 2][:, (h % 2) * 256:(h % 2) * 256 + 128], scalar=DD[:, h:h + 1], in1=hbuf[:, gl, h, :], op0=ALU.mult, op1=ALU.add)),
                 r=[pO[h // 2], DD, (hbuf, gl, h)], w=[(HS, h)])
            P.op("act", (lambda e, h=h: e.activation(out=hjunk[:], in_=HS[:, h, :], func=AF.Square, accum_out=SSN[:, h:h + 1])),
                 r=[(HS, h)], w=[hjunk, (SSN, h)])
        P.op("act", (lambda e: e.activation(out=SSN[:], in_=SSN[:], func=AF.Sqrt, bias=epsc[:, 0:1], scale=1.0 / 128)),
             r=[(SSN, h) for h in range(4)] + epskeys, w=[SSN])
        P.op("dve", (lambda e: e.reciprocal(out=SSN[:], in_=SSN[:])), r=[SSN], w=[SSN])
        for h in range(4):
            P.op("dve", (lambda e, h=h: e.scalar_tensor_tensor(out=HS[:, h, :], in0=HS[:, h, :], scalar=SSN[:, h:h + 1], in1=gmlrep[:, h * 128:(h + 1) * 128], op0=ALU.mult, op1=ALU.mult)),
                 r=[(HS, h), SSN, gmlrep], w=[(HS, h)])
        P.op("pool", (lambda e: e.tensor_tensor(out=MB[:], in0=HS[:].rearrange("p h c -> p (h c)"), in1=SG[:], op=ALU.mult)),
             r=[(HS, h) for h in range(4)] + [SG], w=[MB])
        pTr = PS[6 + fi]
        ptb = pTr[:].bitcast(BF16)
        for h in range(4):
            P.op("pe", (lambda e, h=h: e.transpose(out=ptb[:, h * 128:(h + 1) * 128], in_=MB[:, h * 128:(h + 1) * 128], identity=ident_b[:])),
                 r=[MB, ident_b], w=[pTr])
        P.op("act", (lambda e: e.activation(out=MT[:].rearrange("p h c -> p (h c)"), in_=ptb[:, 0:512], func=AF.Copy)), r=[pTr], w=[MT])
        P.dma("sp", mixT_d[4:8, :, gl * 128:(gl + 1) * 128].rearrange("h p t -> p h t"), MT[:], r=[MT], w=["mix_ml"], nowaw=True)

    for d in range(2):
        ml_load(d, 0)
    for j in range(min(NTILE, d_steps)):
        for d in range(2):
            if j + 1 < NTILE:
                ml_load(d, j + 1)
            ml_step(d, j)
    if "Cst_dbg" in debug:
        Cst_dbg = dscratch("Cst_dbg", [2, 64, 4, 132], F32)
        for d in range(2):
            P.dma("sp", Cst_dbg[d], Cst[d][:], r=[(Cst[d], h) for h in range(4)], w=["mix_ml"], nowaw=True)
    P.flush()
    esD.close()
    if stop_after == "D":
        P.op("sp", lambda e: e.nop(), r=["mix_ml"])
        P.flush(final_wait_keys=["mix_ml"])
        es.close()
        return nc

    esE = ExitStack()

    def sbE(name, shape, dt):
        return esE.enter_context(nc.sbuf_tensor(name, list(shape), dt))

    KnT = sbE("KnT", [128, 4, NT], BF16)
    KrT = sbE("KrT", [64, 4, NT], BF16)
    Vsb = sbE("Vsb", [128, NTILE, 512], BF16)
    w_out_b = sbE("w_out_b", [128, 8, D], BF16)
    grow = sbE("grow", [1, 2, 192], F32)
    gmx = sbE("gmx", [1, 4], F32)
    negC = sbE("negC", [128, 1], F32)
    g2rep = sbE("g2rep", [128, D], F32)
    tmpE = sbE("tmpE", [128, D], F32)
    cols2 = sbE("cols2", [128, 2, 8], F32)
    for h in range(4):
        P.dma("sp", KnT[:, h, :], KnT_d[h], r=["mla_scr"], w=[(KnT, h)], semkey="KT_ld", nowaw=True)
        P.dma("act", KrT[:, h, :], KrT_d[h], r=["mla_scr"], w=[(KrT, h)], semkey="KT_ld", nowaw=True)
    kkeys = [(KnT, h) for h in range(4)] + [(KrT, h) for h in range(4)]
    P.dma("sp", Vsb[:], Vt_d.rearrange("(n p) c -> p n c", p=128), r=["mla_scr"], w=[Vsb])
    w_out_v = w_out_d.rearrange("(k p) n -> p k n", p=128)
    for kc in range(8):
        P.dma("pool", w_out_b[:, kc, :], w_out_v[:, kc, :], w=[(w_out_b, kc)], semkey="w_out_ld", nowaw=True)
    wokeys = [(w_out_b, kc) for kc in range(8)]
    P.dma("sp", grow[0:1, 0, :], g_qn_d.rearrange("(o n) -> o n", o=1), w=[(grow, 0)], semkey="misc_ld", nowaw=True)
    P.dma("sp", grow[0:1, 1, :], g_kn_d.rearrange("(o n) -> o n", o=1), w=[(grow, 1)], semkey="misc_ld", nowaw=True)
    P.dma("sp", g2rep[:], g2_d.rearrange("(o n) -> o n", o=1).to_broadcast([128, D]), w=[g2rep], semkey="misc_ld", nowaw=True)
    P.op("act", lambda e: e.activation(out=grow[:], in_=grow[:], func=AF.Abs), r=[(grow, 0), (grow, 1)], w=[grow])
    P.op("dve", lambda e: e.tensor_reduce(out=gmx[0:1, 0:2], in_=grow[:], axis=AX.X, op=ALU.max), r=[grow], w=[(gmx, 0)])
    P.op("dve", lambda e: e.tensor_tensor(out=gmx[0:1, 2:3], in0=gmx[0:1, 0:1], in1=gmx[0:1, 1:2], op=ALU.mult), r=[(gmx, 0)], w=[(gmx, 1)])
    P.op("dve", lambda e: e.tensor_scalar(out=gmx[0:1, 3:4], in0=gmx[0:1, 2:3], scalar1=-math.sqrt(192.0), scalar2=None, op0=ALU.mult), r=[(gmx, 1)], w=[(gmx, 2)])
    P.op("pe", lambda e: e.matmul(PS[7][:, 0:1], lhsT=ones_f[0:1, :], rhs=gmx[0:1, 3:4], start=True, stop=True), r=[ones_f, (gmx, 2)], w=[PS[7]])
    P.op("dve", lambda e: e.tensor_copy(out=negC[:], in_=PS[7][:, 0:1]), r=[PS[7]], w=[negC])
    modkeys = [(modrep, j) for j in range(12)]
    P.op("dve", lambda e: e.scalar_tensor_tensor(out=g2rep[:], in0=modrep[:, 4 * D:5 * D], scalar=1.0, in1=g2rep[:], op0=ALU.add, op1=ALU.mult),
         r=modkeys + [g2rep], w=[g2rep])
    diag_extract(cols2[:, 0, :], g2rep[:], [g2rep], (cols2, 0), tmpE)
    diag_extract(cols2[:, 1, :], modrep[:, 3 * D:4 * D], modkeys, (cols2, 1), tmpE)

    if stop_after == "E0":
        e0_dbg = dscratch("e0_dbg", [128, 16], F32)
        e1_dbg = dscratch("e1_dbg", [128, 1], F32)
        P.dma("sp", e1_dbg[:, :], negC[:], r=[negC], w=["e0_dbg"], nowaw=True)
        P.dma("sp", e0_dbg[:, :], cols2[:].rearrange("p a k -> p (a k)"), r=[(cols2, 0), (cols2, 1)], w=["e0_dbg"], nowaw=True)
        P.op("sp", lambda e: e.nop(), r=["e0_dbg"] + kkeys + [Vsb] + wokeys)
        P.flush(final_wait_keys=["e0_dbg"])
        esE.close()
        es.close()
        return nc
    Qn = [sbE("Qn%d" % i, [128, 512], BF16) for i in range(2)]
    Qr = [sbE("Qr%d" % i, [64, 512], BF16) for i in range(2)]
    PTt = [sbE("PTt%d" % i, [128, 512], BF16) for i in range(3)]
    rden = sbE("rden", [128, 512], F32)
    mixa = [sbE("mixa%d" % i, [128, 4, 512], BF16) for i in range(1)] * 2
    mixm = [sbE("mixm%d" % i, [128, 4, 512], BF16) for i in range(1)] * 2
    xE = [sbE("xE%d" % i, [128, D], F32) for i in range(1)] * 2
    x1blk = sbE("x1blk", [128, 4, D], F32)
    xsblk = sbE("xsblk", [128, 4, D], BF16)
    h2blk = sbE("h2blk", [128, 8, 512], BF16)
    ssblk = sbE("ssblk", [128, 8], F32)
    ecnt = {"q": 0, "pt": 0, "t": 0}

    def attn_block(qb):
        q0 = qb * 512
        MA = mixa[qb % 2]
        MM = mixm[qb % 2]
        P.dma("act", MM[:], mixT_d[4:8, :, q0:q0 + 512].rearrange("h p t -> p h t"), r=["mix_ml"], w=[MM])
        for h in range(4):
            ecnt["q"] += 1
            QN, QR = Qn[ecnt["q"] % 2], Qr[ecnt["q"] % 2]
            P.dma("sp", QN[:], QnT_d[h, :, q0:q0 + 512], r=["mla_scr"], w=[QN])
            P.dma("sp", QR[:], QrT_d[h, :, q0:q0 + 512], r=["mla_scr"], w=[QR])
            pO, pDn = PS[2 + (ecnt["q"] % 2)], PS[4 + (ecnt["q"] % 2)]
            for kt in range(NTILE):
                pS = PS[kt % 2]
                ks = slice(kt * 128, (kt + 1) * 128)
                P.op("pe", (lambda e, pS=pS, ks=ks, h=h, QN=QN: e.matmul(pS[:], lhsT=KnT[:, h, ks], rhs=QN[:], start=True, stop=False)), r=[(KnT, h), QN], w=[pS])
                P.op("pe", (lambda e, pS=pS, ks=ks, h=h, QR=QR: e.matmul(pS[:], lhsT=KrT[:, h, ks], rhs=QR[:], start=False, stop=True)), r=[(KrT, h), QR], w=[pS])
                if elevel < 1:
                    continue
                ecnt["pt"] += 1
                PT = PTt[ecnt["pt"] % 3]
                P.op("act", (lambda e, pS=pS, PT=PT: e.activation(out=PT[:], in_=pS[:], func=AF.Exp, bias=negC[:], scale=1.0)), r=[pS, negC], w=[PT])
                if elevel < 2:
                    continue
                P.op("pe", (lambda e, PT=PT, kt=kt, h=h, pO=pO: e.matmul(pO[:], lhsT=Vsb[:, kt, h * 128:(h + 1) * 128], rhs=PT[:], start=(kt == 0), stop=(kt == NTILE - 1))),
                     r=[Vsb, PT], w=[pO])
                P.op("pe", (lambda e, PT=PT, kt=kt, pDn=pDn: e.matmul(pDn[:], lhsT=ones_b[:], rhs=PT[:], start=(kt == 0), stop=(kt == NTILE - 1))),
                     r=[ones_b, PT], w=[pDn])
            if elevel < 3:
                continue
            P.op("dve", (lambda e, pDn=pDn: e.reciprocal(out=rden[:], in_=pDn[:])), r=[pDn], w=[rden], name="rdenop")
            P.op("dve", (lambda e, pO=pO, h=h: e.scalar_tensor_tensor(out=MA[:, h, :], in0=pO[:], scalar=1.0, in1=rden[:], op0=ALU.mult, op1=ALU.mult)), r=[pO, rden], w=[(MA, h)], name="MAop")
        if "mixa_dbg" in debug:
            for h in range(4):
                P.op("act", (lambda e, h=h: e.activation(out=PTt[h % 3][:], in_=MA[:, h, :], func=AF.Copy)), r=[(MA, h)], w=[PTt[h % 3]], name="MAcopy")
                P.dma("sp", mixT_d[h, :, q0:q0 + 512], PTt[h % 3][:], r=[PTt[h % 3]], w=["x1_scr"], nowaw=True)
        X1B, XSB, H2B, SSB = x1blk, xsblk, h2blk, ssblk

        def out_tile(i):
            ecnt["t"] += 1
            ti = ecnt["t"] % 2
            tt = q0 + i * 128
            XE = xE[ti]
            P.dma("act", XE[:], x_d[tt:tt + 128, :], w=[XE])
            pY = [PS[6], PS[7]]
            for half in range(2):
                for kc in range(8):
                    src = MA if kc < 4 else MM
                    P.op("pe", (lambda e, half=half, kc=kc, src=src, i=i: e.matmul(pY[half][:], lhsT=src[:, kc % 4, i * 128:(i + 1) * 128], rhs=w_out_b[:, kc, half * 512:(half + 1) * 512],
                                                                              start=(kc == 0), stop=(kc == 7))),
                         r=[(MA, h) for h in range(4)] + [MM] + wokeys, w=[pY[half]])
            for half in range(2):
                hs_ = slice(half * 512, (half + 1) * 512)
                P.op("dve", (lambda e, half=half, hs_=hs_: e.scalar_tensor_tensor(out=X1B[:, i, hs_], in0=pY[half][:], scalar=1.0, in1=modrep[:, 2 * D + half * 512:2 * D + (half + 1) * 512], op0=ALU.mult, op1=ALU.mult)),
                     r=[pY[half]] + modkeys, w=[(X1B, i, half)])
                P.op("pool", (lambda e, hs_=hs_: e.tensor_tensor(out=X1B[:, i, hs_], in0=X1B[:, i, hs_], in1=XE[:, hs_], op=ALU.add)), r=[(X1B, i, half), XE], w=[(X1B, i, half)])
            x1keys = [(X1B, i, 0), (X1B, i, 1)]
            P.dma("sp", x1_d[tt:tt + 128, :], X1B[:, i, :], r=x1keys, w=["x1_scr"], nowaw=True)
            P.op("act", (lambda e: e.activation(out=tmpE[:].bitcast(BF16)[:, 0:D], in_=X1B[:, i, :], func=AF.Square, accum_out=SSB[:, i:i + 1])), r=x1keys, w=[tmpE, (SSB, i)])

        def norm_block():
            allx1 = [(X1B, i, hf) for i in range(4) for hf in range(2)]
            P.op("act", (lambda e: e.activation(out=SSB[:, 4:8], in_=SSB[:, 0:4], func=AF.Sqrt, bias=epsc[:, 0:1], scale=1.0 / D)), r=[(SSB, i) for i in range(4)] + epskeys, w=[(SSB, "r")])
            P.op("dve", (lambda e: e.reciprocal(out=SSB[:, 4:8], in_=SSB[:, 4:8])), r=[(SSB, "r")], w=[(SSB, "r")])
            for i in range(4):
                P.op("dve", (lambda e, i=i: e.tensor_scalar(out=XSB[:, i, :], in0=X1B[:, i, :], scalar1=SSB[:, 4 + i:5 + i], scalar2=None, op0=ALU.mult)),
                     r=allx1 + [(SSB, "r")], w=[(XSB, i)])
            for kc in range(8):
                pt = PS[kc % 2]
                ptb = pt[:].bitcast(BF16)
                for i in range(4):
                    P.op("pe", (lambda e, ptb=ptb, i=i, kc=kc: e.transpose(out=ptb[:, i * 128:(i + 1) * 128], in_=XSB[:, i, kc * 128:(kc + 1) * 128], identity=ident_b[:])),
                         r=[(XSB, i), ident_b], w=[pt])
                if kc % 2 == 0:
                    P.op("act", (lambda e, ptb=ptb, kc=kc: e.activation(out=H2B[:, kc, :], in_=ptb[:, 0:512], func=AF.Identity,
                                                                      scale=cols2[:, 0, kc:kc + 1], bias=cols2[:, 1, kc:kc + 1])),
                         r=[pt, (cols2, 0), (cols2, 1)], w=[(H2B, kc)])
                else:
                    P.op("dve", (lambda e, ptb=ptb, kc=kc: e.tensor_scalar(out=H2B[:, kc, :], in0=ptb[:, 0:512],
                                                                         scalar1=cols2[:, 0, kc:kc + 1], scalar2=cols2[:, 1, kc:kc + 1], op0=ALU.mult, op1=ALU.add)),
                         r=[pt, (cols2, 0), (cols2, 1)], w=[(H2B, kc)])
            for kc in range(8):
                P.dma("sp", h2T_d[kc, :, q0:q0 + 512], H2B[:, kc, :], r=[(H2B, kc)], w=["x1_scr"], nowaw=True)

        if "skip_tiles" not in debug:
            for i in range(4):
                out_tile(i)
            if elevel >= 6:
                norm_block()

    for qb in range(e_blocks):
        attn_block(qb)
    P.flush()
    esE.close()
    if stop_after == "E":
        P.op("sp", lambda e: e.nop(), r=["x1_scr"])
        P.flush(final_wait_keys=["x1_scr"])
        es.close()
        return nc

    esF = ExitStack()

    def sbF(name, shape, dt):
        return esF.enter_context(nc.sbuf_tensor(name, list(shape), dt))

    NEG = -1.0e30
    w_pq_b = sbF("w_pq_b", [128, 8, 2048], BF16)
    Ksf = sbF("Ksf", [128, 16, 128], F32)
    KsT = sbF("KsT", [128, 16, 128], BF16)
    iota_f = sbF("iota_f", [128, 128], F32)
    thr = sbF("thr", [128, 16], F32)
    h2g = sbF("h2g", [128, 8, 512], BF16)
    qTs = sbF("qTs", [128, 16, 512], BF16)
    Ssc = sbF("Ssc", [128, 16, 128], F32)
    sv = sbF("sv", [128, 16, 16], F32)
    siu = sbF("siu", [128, 16, 16], U32)
    sif = sbF("sif", [128, 16, 16], F32)
    cand = sbF("cand", [128, 8, 256], F32)
    best = sbF("best", [128, 8, 16], F32)
    posu = sbF("posu", [128, 8, 16], U32)
    posf = sbF("posf", [128, 8, 16], F32)
    ak = sbF("ak", [128, 8, 16], F32)
    bk = sbF("bk", [128, 8, 16], F32)
    big = sbF("big", [128, 8, 16, 16], F32)
    sel3 = sbF("sel3", [128, 3, 128], F32)
    zs = sbF("zs", [128, 8], F32)
    T3 = sbF("T3", [128, 3, 128], F32)
    Roh2 = [sbF("Roh%d" % i, [128, 64, 128], BF16) for i in range(2)]
    Loh2 = [sbF("Loh%d" % i, [128, 64, 128], BF16) for i in range(2)]
    Wsb = [sbF("Wsb%d" % i, [128, 128, 64], BF16) for i in range(1)] * 2
    w_pq_v = w_pq_d.rearrange("(k p) n -> p k n", p=128)
    for kc in range(8):
        P.dma("pool", w_pq_b[:, kc, :], w_pq_v[:, kc, :], w=[(w_pq_b, kc)], semkey="w_pq_ld", nowaw=True)
    wpqkeys = [(w_pq_b, kc) for kc in range(8)]
    P.dma("sp", Ksf[:], subk_d.rearrange("g k d -> k g d"), w=[Ksf], semkey="misc_ld", nowaw=True)
    P.dma("sp", iota_f[:], iota_d, w=[iota_f], semkey="misc_ld", nowaw=True)
    P.op("dve", lambda e: e.tensor_scalar(out=thr[:, 0:15], in0=iota_f[:, 1:16], scalar1=16.0, scalar2=None, op0=ALU.mult), r=[iota_f], w=[thr])
    for g4 in range(4):
        pt = PS[g4]
        for i in range(4):
            P.op("pe", (lambda e, pt=pt, i=i, g4=g4: e.transpose(out=pt[:, i * 128:(i + 1) * 128], in_=Ksf[:, g4 * 4 + i, :], identity=ident_f[:])), r=[Ksf, ident_f], w=[pt])
        P.op("act", (lambda e, pt=pt, g4=g4: e.activation(out=KsT[:, g4 * 4:g4 * 4 + 4, :].rearrange("p g k -> p (g k)"), in_=pt[:], func=AF.Copy)), r=[pt], w=[(KsT, g4)])
    kstkeys = [(KsT, g4) for g4 in range(4)]
    fcnt = {"w": 0}

    def route_group(gq):
        t0g = gq * 512
        P.dma("sp", h2g[:], h2T_d[:, :, t0g:t0g + 512].rearrange("k p t -> p k t"), r=["x1_scr"], w=[h2g])
        for hp in range(16):
            pt = PS[hp % 2]
            for kc in range(8):
                P.op("pe", (lambda e, pt=pt, hp=hp, kc=kc: e.matmul(pt[:], lhsT=w_pq_b[:, kc, hp * 128:(hp + 1) * 128], rhs=h2g[:, kc, :], start=(kc == 0), stop=(kc == 7))),
                     r=wpqkeys + [h2g], w=[pt])
            if hp % 2 == 0:
                P.op("act", (lambda e, pt=pt, hp=hp: e.activation(out=qTs[:, hp, :], in_=pt[:], func=AF.Copy)), r=[pt], w=[(qTs, hp)])
            else:
                P.op("dve", (lambda e, pt=pt, hp=hp: e.tensor_copy(out=qTs[:, hp, :], in_=pt[:])), r=[pt], w=[(qTs, hp)])
        for tl in range(4):
            route_tile(gq * 4 + tl, tl)

    def route_tile(tile, tl):
        qkeys = [(qTs, hp) for hp in range(16)]
        for g4 in range(4):
            pt = PS[2 + g4]
            for i in range(4):
                hp = g4 * 4 + i
                P.op("pe", (lambda e, pt=pt, i=i, hp=hp: e.matmul(pt[:, i * 128:(i + 1) * 128], lhsT=qTs[:, hp, tl * 128:(tl + 1) * 128], rhs=KsT[:, hp, :], start=True, stop=True)),
                     r=qkeys + kstkeys, w=[pt])
            if g4 % 2 == 0:
                P.op("act", (lambda e, pt=pt, g4=g4: e.activation(out=Ssc[:, g4 * 4:g4 * 4 + 4, :].rearrange("p g k -> p (g k)"), in_=pt[:], func=AF.Copy)), r=[pt], w=[(Ssc, g4)])
            else:
                P.op("dve", (lambda e, pt=pt, g4=g4: e.tensor_copy(out=Ssc[:, g4 * 4:g4 * 4 + 4, :].rearrange("p g k -> p (g k)"), in_=pt[:])), r=[pt], w=[(Ssc, g4)])
        for hp in range(16):
            k_ = (Ssc, hp // 4)
            for rnd in range(2):
                sl = slice(rnd * 8, rnd * 8 + 8)
                P.op("dve", (lambda e, hp=hp, sl=sl: e.max(out=sv[:, hp, sl], in_=Ssc[:, hp, :])), r=[k_], w=[(sv, hp)])
                P.op("dve", (lambda e, hp=hp, sl=sl: e.max_index(out=siu[:, hp, sl], in_max=sv[:, hp, sl], in_values=Ssc[:, hp, :])), r=[k_, (sv, hp)], w=[(siu, hp)])
                if rnd == 0:
                    P.op("dve", (lambda e, hp=hp, sl=sl: e.match_replace(out=Ssc[:, hp, :], in_to_replace=sv[:, hp, sl], in_values=Ssc[:, hp, :], imm_value=NEG)), r=[k_, (sv, hp)], w=[k_])
        svkeys = [(sv, hp) for hp in range(16)]
        sikeys = [(siu, hp) for hp in range(16)]
        P.op("dve", lambda e: e.tensor_copy(out=sif[:], in_=siu[:]), r=sikeys, w=[sif])
        svv = sv[:].rearrange("p (h q) a -> p h q a", q=2)
        sfv = sif[:].rearrange("p (h q) a -> p h q a", q=2)
        P.op("dve", lambda e: e.tensor_tensor(out=cand[:].rearrange("p h (a b) -> p h a b", b=16), in0=svv[:, :, 0, :].unsqueeze(3).to_broadcast([128, 8, 16, 16]),
                                              in1=svv[:, :, 1, :].unsqueeze(2).to_broadcast([128, 8, 16, 16]), op=ALU.add), r=svkeys, w=[cand])
        for h in range(8):
            for rnd in range(2):
                sl = slice(rnd * 8, rnd * 8 + 8)
                P.op("dve", (lambda e, h=h, sl=sl: e.max(out=best[:, h, sl], in_=cand[:, h, :])), r=[cand], w=[(best, h)])
                P.op("dve", (lambda e, h=h, sl=sl: e.max_index(out=posu[:, h, sl], in_max=best[:, h, sl], in_values=cand[:, h, :])), r=[cand, (best, h)], w=[(posu, h)])
                if rnd == 0:
                    P.op("dve", (lambda e, h=h, sl=sl: e.match_replace(out=cand[:, h, :], in_to_replace=best[:, h, sl], in_values=cand[:, h, :], imm_value=NEG)), r=[cand, (best, h)], w=[cand])
        bkeys = [(best, h) for h in range(8)]
        pkeys = [(posu, h) for h in range(8)]
        g3 = sel3[:, 2, :].rearrange("p (h k) -> p h k", k=16)
        P.op("dve", lambda e: e.tensor_tensor(out=g3, in0=best[:], in1=best[:, :, 0:1].to_broadcast([128, 8, 16]), op=ALU.subtract), r=bkeys, w=[(sel3, 2)])
        P.op("act", lambda e: e.activation(out=g3, in_=g3, func=AF.Exp), r=[(sel3, 2)], w=[(sel3, 2)])
        P.op("dve", lambda e: e.tensor_reduce(out=zs[:], in_=g3, axis=AX.X, op=ALU.add), r=[(sel3, 2)], w=[zs])
        P.op("dve", lambda e: e.reciprocal(out=zs[:], in_=zs[:]), r=[zs], w=[zs])
        P.op("dve", lambda e: e.tensor_tensor(out=g3, in0=g3, in1=zs[:].unsqueeze(2).to_broadcast([128, 8, 16]), op=ALU.mult), r=[(sel3, 2), zs], w=[(sel3, 2)])
        P.op("dve", lambda e: e.tensor_copy(out=posf[:], in_=posu[:]), r=pkeys, w=[posf])
        P.op("dve", lambda e: e.tensor_tensor(out=big[:, :, :, 0:15], in0=posf[:].unsqueeze(3).to_broadcast([128, 8, 16, 15]),
                                              in1=thr[:, 0:15].unsqueeze(1).unsqueeze(1).to_broadcast([128, 8, 16, 15]), op=ALU.is_ge), r=[posf, thr], w=[big])
        P.op("dve", lambda e: e.tensor_reduce(out=ak[:], in_=big[:, :, :, 0:15], axis=AX.X, op=ALU.add), r=[big], w=[ak])
        P.op("dve", lambda e: e.scalar_tensor_tensor(out=bk[:], in0=ak[:], scalar=-16.0, in1=posf[:], op0=ALU.mult, op1=ALU.add), r=[ak, posf], w=[bk])
        for (src, q, dsti) in ((ak, 0, 0), (bk, 1, 1)):
            P.op("dve", (lambda e, src=src: e.tensor_tensor(out=big[:], in0=src[:].unsqueeze(3).to_broadcast([128, 8, 16, 16]),
                                                           in1=iota_f[:, 0:16].unsqueeze(1).unsqueeze(1).to_broadcast([128, 8, 16, 16]), op=ALU.is_equal)), r=[src, iota_f, big], w=[big])
            P.op("dve", (lambda e, q=q: e.tensor_tensor(out=big[:], in0=big[:], in1=sfv[:, :, q, :].unsqueeze(2).to_broadcast([128, 8, 16, 16]), op=ALU.mult)), r=[big, sif], w=[big])
            P.op("dve", (lambda e, dsti=dsti: e.tensor_reduce(out=sel3[:, dsti, :].rearrange("p (h k) -> p h k", k=16), in_=big[:], axis=AX.X, op=ALU.add)), r=[big], w=[(sel3, dsti)])
        ptT = PS[6]
        for c3 in range(3):
            P.op("pe", (lambda e, c3=c3: e.transpose(out=ptT[:, c3 * 128:(c3 + 1) * 128], in_=sel3[:, c3, :], identity=ident_f[:])), r=[(sel3, c3), ident_f], w=[ptT])
        P.op("act", lambda e: e.activation(out=T3[:].rearrange("p c t -> p (c t)"), in_=ptT[:, 0:384], func=AF.Copy), r=[ptT], w=[T3])
        for half in range(2):
            hs_ = slice(half * 64, half * 64 + 64)
            Roh, Loh = Roh2[half], Loh2[half]
            onehot_half(half, hs_, Roh, Loh, tile)

    def onehot_half(half, hs_, Roh, Loh, tile):
        if True:
            P.op("dve", (lambda e, hs_=hs_: e.tensor_tensor(out=Roh[:], in0=iota_f[:].unsqueeze(1).to_broadcast([128, 64, 128]),
                                                           in1=T3[:, 0, hs_].unsqueeze(2).to_broadcast([128, 64, 128]), op=ALU.is_equal)), r=[T3, iota_f], w=[Roh])
            P.op("dve", (lambda e, hs_=hs_: e.tensor_tensor(out=Loh[:], in0=iota_f[:].unsqueeze(1).to_broadcast([128, 64, 128]),
                                                           in1=T3[:, 1, hs_].unsqueeze(2).to_broadcast([128, 64, 128]), op=ALU.is_equal)), r=[T3, iota_f], w=[Loh])
            P.op("dve", (lambda e, hs_=hs_: e.tensor_tensor(out=Loh[:], in0=Loh[:], in1=T3[:, 2, hs_].unsqueeze(2).to_broadcast([128, 64, 128]), op=ALU.mult)), r=[T3, Loh], w=[Loh])
            fcnt["w"] += 1
            WS = Wsb[fcnt["w"] % 2]
            for t4 in range(16):
                pw = PS[t4 % 2]
                for u in range(4):
                    t = t4 * 4 + u
                    P.op("pe", (lambda e, pw=pw, u=u, t=t: e.matmul(pw[:, u * 128:(u + 1) * 128], lhsT=Loh[:, t, :], rhs=Roh[:, t, :], start=True, stop=True)), r=[Loh, Roh], w=[pw])
                outv = WS[:, :, t4 * 4:t4 * 4 + 4].rearrange("j i t -> j t i")
                inv = pw[:].rearrange("j (t i) -> j t i", i=128)
                if True:
                    P.op("act", (lambda e, outv=outv, inv=inv: e.activation(out=outv, in_=inv, func=AF.Copy)), r=[pw], w=[(WS, t4)])
                else:
                    P.op("dve", (lambda e, outv=outv, inv=inv: e.tensor_copy(out=outv, in_=inv)), r=[pw], w=[(WS, t4)])
            P.dma("sp", W_d[tile, half], WS[:], r=[(WS, t4) for t4 in range(16)], w=["W_scr"], nowaw=True)

    for gq in range(f_groups):
        route_group(gq)
    P.flush()
    esF.close()
    if stop_after == "F":
        P.op("sp", lambda e: e.nop(), r=["W_scr"])
        P.flush(final_wait_keys=["W_scr"])
        es.close()
        return nc

    esG = ExitStack()

    def sbG(name, shape, dt):
        return esG.enter_context(nc.sbuf_tensor(name, list(shape), dt))

    Uc = [sbG("Uc%d" % i, [128, D], BF16) for i in range(2)]
    UTs = [sbG("UTs%d" % i, [128, 8, 1024], BF16) for i in range(2)]
    Vs = [sbG("Vs%d" % i, [128, 8, D], BF16) for i in range(2)]
    Wsl = [sbG("Wsl%d" % i, [128, 8, 8, 128], BF16) for i in range(2)]
    h2sg = sbG("h2sg", [128, 8, 1024], BF16)
    acc = sbG("acc", [128, 8, D], F32)
    Gt_ = [sbG("Gt%d" % i, [128, 256], F32) for i in range(2)]
    PTg = [sbG("PTg%d" % i, [128, 256], BF16) for i in range(2)]
    x1g = sbG("x1g", [128, D], F32)
    og = sbG("og", [128, D], F32)
    gcnt = {"u": 0, "a": 0}
    modkeys = [(modrep, j) for j in range(12)]

    def expert_super(sg, sc):
        UT, V, W = UTs[sc % 2], Vs[sc % 2], Wsl[sc % 2]
        P.dma("pool", V[:], ev_d[sc * 1024:(sc + 1) * 1024, :].rearrange("(c p) d -> p c d", p=128), w=[V])
        for tile in range(8):
            for half in range(2):
                P.dma("sp" if half == 0 else "act", W[:, :, tile, half * 64:(half + 1) * 64], W_d[sg * 8 + tile, half, :, sc * 8:(sc + 1) * 8, :],
                      r=["W_scr"], w=[(W, tile, half)], semkey=(W, "ld"), nowaw=True)
        wkeys = [(W, tile, half) for tile in range(8) for half in range(2)]
        utkeys = [(UT, ci, q) for ci in range(8) for q in range(2)]
        if sg > 0:
            P.dma("sp", UT[:], UT_d[sc], r=["UT_scr"], w=utkeys, semkey=(UT, "ld"))
        for ci in (range(8) if sg == 0 else []):
            c = sc * 8 + ci
            gcnt["u"] += 1
            U_ = Uc[gcnt["u"] % 2]
            P.dma("pool", U_[:], eu_d[c * 128:(c + 1) * 128, :], w=[U_])
            for q in range(2):
                pt = PS[6 + q]
                ptb = pt[:].bitcast(BF16)
                for k4 in range(4):
                    kc = q * 4 + k4
                    P.op("pe", (lambda e, ptb=ptb, k4=k4, kc=kc, U_=U_: e.transpose(out=ptb[:, k4 * 128:(k4 + 1) * 128], in_=U_[:, kc * 128:(kc + 1) * 128], identity=ident_b[:])),
                         r=[U_, ident_b], w=[pt])
                outv = UT[:, q * 4:q * 4 + 4, ci * 128:(ci + 1) * 128]
                inv = ptb[:, 0:512].rearrange("p (k e) -> p k e", e=128)
                if q == 0:
                    P.op("act", (lambda e, outv=outv, inv=inv: e.activation(out=outv, in_=inv, func=AF.Copy)), r=[pt], w=[(UT, ci, q)])
                else:
                    P.op("dve", (lambda e, outv=outv, inv=inv: e.tensor_copy(out=outv, in_=inv)), r=[pt], w=[(UT, ci, q)])
        if sg == 0 and g_sgs > 1:
            P.dma("act", UT_d[sc], UT[:], r=utkeys, w=["UT_scr"], nowaw=True)
        for tp in range(4):
            for ci in range(8):
                gcnt["a"] += 1
                ai = gcnt["a"] % 2
                pA = PS[4 + ai]
                G_, PT_ = Gt_[ai], PTg[ai]
                for kc in range(8):
                    P.op("pe", (lambda e, pA=pA, kc=kc, ci=ci, tp=tp: e.matmul(pA[:, 0:256], lhsT=UT[:, kc, ci * 128:(ci + 1) * 128], rhs=h2sg[:, kc, tp * 256:(tp + 1) * 256],
                                                                         start=(kc == 0), stop=(kc == 7))),
                         r=[(UT, ci, 0), (UT, ci, 1), h2sg], w=[pA])
                P.op("act", (lambda e, pA=pA, G_=G_: e.activation(out=G_[:], in_=pA[:, 0:256], func=AF.Gelu)), r=[pA], w=[G_])
                P.op("dve", (lambda e, G_=G_, PT_=PT_, ci=ci, tp=tp: e.tensor_tensor(out=PT_[:], in0=G_[:], in1=W[:, ci, 2 * tp:2 * tp + 2, :].rearrange("p a t -> p (a t)"), op=ALU.mult)),
                     r=[G_] + wkeys, w=[PT_])
                for tl in range(2):
                    for h2_ in range(2):
                        pc = PS[tl * 2 + h2_]
                        P.op("pe", (lambda e, pc=pc, PT_=PT_, tl=tl, h2_=h2_, ci=ci: e.matmul(pc[:], lhsT=PT_[:, tl * 128:(tl + 1) * 128], rhs=V[:, ci, h2_ * 512:(h2_ + 1) * 512],
                                                                                     start=(ci == 0), stop=(ci == 7))),
                             r=[PT_, V], w=[pc])
            for tl in range(2):
                tile = tp * 2 + tl
                for h2_ in range(2):
                    pc = PS[tl * 2 + h2_]
                    dst = acc[:, tile, h2_ * 512:(h2_ + 1) * 512]
                    if sc == 0:
                        P.op("act", (lambda e, pc=pc, dst=dst: e.activation(out=dst, in_=pc[:], func=AF.Copy)), r=[pc], w=[(acc, tile, h2_)])
                    else:
                        P.op("dve", (lambda e, pc=pc, dst=dst: e.scalar_tensor_tensor(out=dst, in0=pc[:], scalar=1.0, in1=dst, op0=ALU.mult, op1=ALU.add)),
                             r=[pc, (acc, tile, h2_)], w=[(acc, tile, h2_)])

    def final_tile(sg, tile):
        tt = sg * 1024 + tile * 128
        P.dma("sp", x1g[:], x1_d[tt:tt + 128, :], r=["x1_scr"], w=[x1g])
        P.op("dve", lambda e: e.tensor_tensor(out=og[:], in0=acc[:, tile, :], in1=modrep[:, 5 * D:6 * D], op=ALU.mult),
             r=[(acc, tile, 0), (acc, tile, 1)] + modkeys, w=[og])
        P.op("pool", lambda e: e.tensor_tensor(out=og[:], in0=og[:], in1=x1g[:], op=ALU.add), r=[og, x1g], w=[og])
        P.dma("sp", out_d[tt:tt + 128, :], og[:], r=[og], w=["out"], nowaw=True)

    for sg in range(g_sgs):
        P.dma("sp", h2sg[:], h2T_d[:, :, sg * 1024:(sg + 1) * 1024].rearrange("k p t -> p k t"), r=["x1_scr"], w=[h2sg])
        for sc in range(g_scs):
            expert_super(sg, sc)
        for tile in range(8):
            final_tile(sg, tile)
    P.op("sp", lambda e: e.nop(), r=["out"])
    P.flush(final_wait_keys=["out"])
    esG.close()
    es.close()
    return nc


def make_in_maps(inputs):
    consts = _host_consts()
    maps = []
    for b in range(8):
        m = {
            "x": np.ascontiguousarray(inputs["x"][b]),
            "ctx": np.ascontiguousarray(inputs["ctx"][b]),
            "c": np.ascontiguousarray(inputs["c"][b]),
            "c_ctx": np.ascontiguousarray(inputs["c_ctx"]),
            "w_ada": np.ascontiguousarray(inputs["w_ada"][0]),
            "b_ada": np.ascontiguousarray(inputs["b_ada"][0]),
            "g_norm1": np.ascontiguousarray(inputs["g_norm1"][0]),
            "w_in": np.ascontiguousarray(inputs["w_in"][0]),
            "g_cq": np.ascontiguousarray(inputs["g_cq"][0]),
            "w_uq": np.ascontiguousarray(inputs["w_uq"][0]),
            "g_ckv": np.ascontiguousarray(inputs["g_ckv"][0]),
            "w_ukv": np.ascontiguousarray(inputs["w_ukv"][0]),
            "g_qn": np.ascontiguousarray(inputs["g_qn"][0]),
            "g_kn": np.ascontiguousarray(inputs["g_kn"][0]),
            "conv_qk": np.ascontiguousarray(inputs["conv_qk"][0]),
            "b_igate": np.ascontiguousarray(inputs["b_igate"][0].reshape(8)),
            "b_fgate": np.ascontiguousarray(inputs["b_fgate"][0].reshape(8)),
            "g_mlstm": np.ascontiguousarray(inputs["g_mlstm"][0]),
            "w_out": np.ascontiguousarray(inputs["w_out"][0]),
            "w_pq": np.ascontiguousarray(inputs["w_pq"][0]),
            "sub_keys": np.ascontiguousarray(inputs["sub_keys"][0].reshape(16, 128, 128)),
            "expert_u": np.ascontiguousarray(inputs["expert_u"][0]),
            "expert_v": np.ascontiguousarray(inputs["expert_v"][0]),
            "iota": consts["iota"],
            "g_norm2": np.ascontiguousarray(inputs["g_norm2"][0]),
            "mask_f": consts["mask_f"],
            "mask_b": consts["mask_b"],
            "ident": consts["ident"],
            "cosT": consts["cosT"],
            "sinT": consts["sinT"],
        }
        maps.append(m)
    return maps


def kernel(**inputs):
    nc = build_program()
    in_maps = make_in_maps(inputs)
    res = run_bass_kernel_spmd(nc, in_maps, core_ids=list(range(8)))
    return np.stack([r["out"] for r in res.results], axis=0)
```
